# Optimizing a Trainium2 kernel written in Bass

```python
import jax, jax.numpy as jnp
from jax import lax
import numpy as np

D_MODEL = 1024
BATCH = 16
SEQ = 4096
DEPTH = 1

D_MIX = D_MODEL
ATTN_WIDTH = D_MIX // 2
ATTN_HEAD_DIM = 64
ATTN_HEADS = ATTN_WIDTH // ATTN_HEAD_DIM
DILATED_PATTERNS = ((128, 1), (512, 4), (2048, 16))
ATTN_BLOCK = 128
GMLP_WIDTH = D_MIX - ATTN_WIDTH
GMLP_CHUNK = 128
GMLP_GROUP = 128
GMLP_GROUPS = GMLP_WIDTH // GMLP_GROUP
IN_WIDTH = 3 * ATTN_WIDTH + 2 * GMLP_WIDTH
PEER_HEADS = 8
PEER_TOPK = 16
N_KEYS = 128
N_EXPERTS = N_KEYS * N_KEYS
D_KEY = 256
PEER_TOKEN_BLOCK = 128
RMS_EPS = 1e-6
NEG = -1e30

kernel_name = "hymba_dilated_gmlp_peer_layer"


def rms_norm(x, g):
    xf = x.astype(jnp.float32)
    y = xf * lax.rsqrt(jnp.mean(xf * xf, axis=-1, keepdims=True) + RMS_EPS)
    return (y * g.astype(jnp.float32)).astype(x.dtype)


def dilated_window_attention(q, k, v, window, dilation):
    b, s, h, c = q.shape
    n_back = window // dilation
    L = s // dilation
    nb = -(-L // ATTN_BLOCK)
    Lp = nb * ATTN_BLOCK

    def to_residue(t):
        return t.reshape(b, L, dilation, h, c).transpose(0, 2, 1, 3, 4)

    qr, kr, vr = to_residue(q), to_residue(k), to_residue(v)
    qb = jnp.pad(qr, ((0, 0), (0, 0), (0, Lp - L), (0, 0), (0, 0))).reshape(b, dilation, nb, ATTN_BLOCK, h, c)

    def key_blocks(t):
        tp = jnp.pad(t, ((0, 0), (0, 0), (ATTN_BLOCK, Lp - L), (0, 0), (0, 0)))
        tp = tp.reshape(b, dilation, nb + 1, ATTN_BLOCK, h, c)
        return jnp.concatenate([tp[:, :, :-1], tp[:, :, 1:]], axis=3)

    kb, vb = key_blocks(kr), key_blocks(vr)
    scores = jnp.einsum('bdnqhc,bdnkhc->bdnhqk', qb, kb).astype(jnp.float32) * (c ** -0.5)
    qi = jnp.arange(ATTN_BLOCK)[:, None]
    kj = jnp.arange(2 * ATTN_BLOCK)[None, :]
    rel = qi + ATTN_BLOCK - kj
    key_pos = jnp.arange(nb)[:, None, None] * ATTN_BLOCK + kj[None] - ATTN_BLOCK
    mask = (rel >= 0) & (rel <= n_back) & (key_pos >= 0)
    scores = jnp.where(mask[:, None], scores, NEG)
    m = jnp.max(scores, axis=-1, keepdims=True)
    p = jnp.exp(scores - m)
    den = jnp.sum(p, axis=-1, keepdims=True)
    out = jnp.einsum('bdnhqk,bdnkhc->bdnqhc', (p / den).astype(v.dtype), vb)
    lse = (m + jnp.log(den))[..., 0]
    out = out.reshape(b, dilation, Lp, h, c)[:, :, :L].transpose(0, 2, 1, 3, 4).reshape(b, s, h, c)
    lse = lse.transpose(0, 1, 2, 4, 3).reshape(b, dilation, Lp, h)[:, :, :L]
    lse = lse.transpose(0, 2, 1, 3).reshape(b, s, h)
    return out, lse


def dilated_mixture_attention(q, k, v):
    outs, lses = [], []
    for window, dilation in DILATED_PATTERNS:
        o, l = dilated_window_attention(q, k, v, window, dilation)
        outs.append(o)
        lses.append(l)
    wts = jax.nn.softmax(jnp.stack(lses, axis=-1), axis=-1)
    return jnp.einsum('bshpc,bshp->bshc', jnp.stack(outs, axis=3), wts.astype(q.dtype))


def chunked_spatial_gating(u, v, v_norm_g, w_spatial, b_spatial):
    b, s, _ = u.shape
    u = jax.nn.gelu(u, approximate=False)
    v = jax.nn.gelu(v, approximate=False)
    v = v.reshape(b, s // GMLP_CHUNK, GMLP_CHUNK, GMLP_GROUPS, GMLP_GROUP)
    v = rms_norm(v, v_norm_g)
    causal = jnp.tril(jnp.ones((GMLP_CHUNK, GMLP_CHUNK), dtype=bool))
    w = jnp.where(causal, w_spatial, jnp.zeros_like(w_spatial)).astype(v.dtype)
    z = jnp.einsum('gij,bnjgc->bnigc', w, v) + b_spatial.T[:, :, None].astype(v.dtype)
    return u * z.reshape(b, s, GMLP_WIDTH)


def peer_ffn(xn, w_query, sub_keys_a, sub_keys_b, expert_u, expert_v):
    b, s, d = xn.shape
    tokens = xn.reshape(-1, PEER_TOKEN_BLOCK, d)
    half = D_KEY // 2

    def block(xb):
        qh = (xb @ w_query).reshape(-1, PEER_HEADS, D_KEY)
        sa = jnp.einsum('thc,kc->thk', qh[..., :half], sub_keys_a).astype(jnp.float32)
        sb = jnp.einsum('thc,kc->thk', qh[..., half:], sub_keys_b).astype(jnp.float32)
        va, ia = lax.top_k(sa, PEER_TOPK)
        vb, ib = lax.top_k(sb, PEER_TOPK)
        cand = (va[..., :, None] + vb[..., None, :]).reshape(-1, PEER_HEADS, PEER_TOPK * PEER_TOPK)
        cand_idx = (ia[..., :, None] * N_KEYS + ib[..., None, :]).reshape(-1, PEER_HEADS, PEER_TOPK * PEER_TOPK)
        top_s, pos = lax.top_k(cand, PEER_TOPK)
        idx = jnp.take_along_axis(cand_idx, pos, axis=-1)
        gate = jax.nn.softmax(top_s, axis=-1)
        u = expert_u[idx]
        act = jax.nn.gelu(jnp.einsum('thkd,td->thk', u, xb), approximate=False)
        coef = (gate * act.astype(jnp.float32)).astype(xb.dtype)
        return jnp.einsum('thk,thkd->td', coef, expert_v[idx])

    return lax.map(block, tokens).reshape(b, s, d)


def setup_inputs(seed: int = 0) -> dict:
    key = jax.random.key(seed)
    ks = jax.random.split(key, 17)

    def nrm(k, shape, scale):
        return jax.random.normal(k, shape, jnp.float32) * scale

    def gain(k, shape):
        return 1.0 + 0.01 * jax.random.normal(k, shape, jnp.float32)

    return {
        "x": nrm(ks[0], (BATCH, SEQ, D_MODEL), 1.0),
        "mix_norm_g": gain(ks[1], (DEPTH, D_MODEL)),
        "w_in": nrm(ks[2], (DEPTH, D_MODEL, IN_WIDTH), D_MODEL ** -0.5),
        "q_norm_g": gain(ks[3], (DEPTH, ATTN_HEAD_DIM)),
        "k_norm_g": gain(ks[4], (DEPTH, ATTN_HEAD_DIM)),
        "v_gate_norm_g": gain(ks[5], (DEPTH, GMLP_GROUPS, GMLP_GROUP)),
        "w_spatial": nrm(ks[6], (DEPTH, GMLP_GROUPS, GMLP_CHUNK, GMLP_CHUNK), GMLP_CHUNK ** -0.5),
        "b_spatial": 1.0 + nrm(ks[7], (DEPTH, GMLP_GROUPS, GMLP_CHUNK), 0.1),
        "attn_out_g": gain(ks[8], (DEPTH, ATTN_WIDTH)),
        "gate_out_g": gain(ks[9], (DEPTH, GMLP_WIDTH)),
        "w_out": nrm(ks[10], (DEPTH, D_MIX, D_MODEL), D_MIX ** -0.5),
        "ffn_norm_g": gain(ks[11], (DEPTH, D_MODEL)),
        "w_query": nrm(ks[12], (DEPTH, D_MODEL, PEER_HEADS * D_KEY), D_MODEL ** -0.5),
        "sub_keys_a": nrm(ks[13], (DEPTH, N_KEYS, D_KEY // 2), (D_KEY // 2) ** -0.5),
        "sub_keys_b": nrm(ks[14], (DEPTH, N_KEYS, D_KEY // 2), (D_KEY // 2) ** -0.5),
        "expert_u": nrm(ks[15], (DEPTH, N_EXPERTS, D_MODEL), D_MODEL ** -0.5),
        "expert_v": nrm(ks[16], (DEPTH, N_EXPERTS, D_MODEL), PEER_HEADS ** -0.5),
    }


def reference(x, mix_norm_g, w_in, q_norm_g, k_norm_g, v_gate_norm_g, w_spatial, b_spatial,
              attn_out_g, gate_out_g, w_out, ffn_norm_g, w_query, sub_keys_a, sub_keys_b,
              expert_u, expert_v):
    b, s, _ = x.shape
    splits = [ATTN_WIDTH, 2 * ATTN_WIDTH, 3 * ATTN_WIDTH, 3 * ATTN_WIDTH + GMLP_WIDTH]
    for l in range(DEPTH):
        hn = rms_norm(x, mix_norm_g[l])
        proj = hn @ w_in[l]
        q, k, v, gu, gv = jnp.split(proj, splits, axis=-1)
        q = rms_norm(q.reshape(b, s, ATTN_HEADS, ATTN_HEAD_DIM), q_norm_g[l])
        k = rms_norm(k.reshape(b, s, ATTN_HEADS, ATTN_HEAD_DIM), k_norm_g[l])
        v = v.reshape(b, s, ATTN_HEADS, ATTN_HEAD_DIM)
        attn = dilated_mixture_attention(q, k, v).reshape(b, s, ATTN_WIDTH)
        gated = chunked_spatial_gating(gu, gv, v_gate_norm_g[l], w_spatial[l], b_spatial[l])
        merged = jnp.concatenate([rms_norm(attn, attn_out_g[l]), rms_norm(gated, gate_out_g[l])], axis=-1)
        x = x + merged @ w_out[l]
        x = x + peer_ffn(rms_norm(x, ffn_norm_g[l]), w_query[l], sub_keys_a[l], sub_keys_b[l],
                         expert_u[l], expert_v[l])
    return x
```

```python
import numpy as np
import ml_dtypes
from contextlib import ExitStack

import concourse.bass as bass
import concourse.mybir as mybir
from concourse.bass_utils import run_bass_kernel_spmd

F32 = mybir.dt.float32
BF = mybir.dt.bfloat16
U32 = mybir.dt.uint32
AF = mybir.ActivationFunctionType
ALU = mybir.AluOpType
AX = mybir.AxisListType

D = 1024
NCH = 128
EPS = 1e-6
N_CORES = 8


class Res:
    __slots__ = ("w", "rd")

    def __init__(self):
        self.w = None
        self.rd = []


class Op:
    __slots__ = ("eng", "fn", "deps", "inc", "semv", "is_dma", "dsem", "dval")

    def __init__(self, eng, fn, deps, is_dma=False):
        self.eng = eng
        self.fn = fn
        self.deps = deps
        self.inc = False
        self.semv = None
        self.is_dma = is_dma
        self.dsem = None
        self.dval = None


class Sched:
    ALL = ("pe", "act", "dve", "pool", "sp")

    def __init__(self, nc, n_dma_sems=24):
        self.nc = nc
        self.ops = {e: [] for e in self.ALL}
        self.sems = {e: nc.alloc_semaphore(f"s_{e}") for e in self.ALL}
        self.dma_sems = [nc.alloc_semaphore(f"s_dma{i}") for i in range(n_dma_sems)]
        self.dma_cnt = [0] * n_dma_sems
        self.dma_last = [None] * n_dma_sems
        self.dma_rr = 0
        self.dry = False

    def _deps(self, eng, reads, writes):
        deps = []
        recent = ()
        if eng != "pe":
            recent = self.ops[eng][-3:]
        for r in reads:
            if r.w is not None:
                deps.append(r.w)
        for w in writes:
            if w.w is not None and (w.w.is_dma or w.w.eng != eng or any(w.w is q for q in recent)):
                deps.append(w.w)
            for o in w.rd:
                if o.is_dma or o.eng != eng or any(o is q for q in recent):
                    deps.append(o)
        return deps

    def _commit(self, op, reads, writes):
        for r in reads:
            r.rd.append(op)
        for w in writes:
            w.w = op
            w.rd = []

    def op(self, eng, fn, reads=(), writes=()):
        if self.dry:
            return None
        o = Op(eng, fn, self._deps(eng, reads, writes))
        self.ops[eng].append(o)
        self._commit(o, reads, writes)
        return o

    def dma(self, fn, reads=(), writes=(), eng="sp"):
        if self.dry:
            return None
        deps = self._deps(eng, reads, writes)
        s = self.dma_rr
        self.dma_rr = (self.dma_rr + 1) % len(self.dma_sems)
        if self.dma_last[s] is not None:
            deps.append(self.dma_last[s])
        o = Op(eng, fn, deps, is_dma=True)
        self.dma_cnt[s] += 1
        o.dsem = s
        o.dval = 16 * self.dma_cnt[s]
        self.dma_last[s] = o
        self.ops[eng].append(o)
        self._commit(o, reads, writes)
        return o

    def barrier(self, engines=None):
        lasts = []
        for e in self.ALL:
            for o in reversed(self.ops[e]):
                if not o.is_dma and o.fn is not None:
                    lasts.append(o)
                    break
        lasts += [o for o in self.dma_last if o is not None]
        for e in (engines or self.ALL):
            self.ops[e].append(Op(e, None, list(lasts)))

    def emit(self, block):
        for e in self.ALL:
            for o in self.ops[e]:
                for d in o.deps:
                    if not d.is_dma:
                        d.inc = True
        for e in self.ALL:
            c = 0
            for o in self.ops[e]:
                if not o.is_dma and o.inc:
                    c += 1
                    o.semv = c
        sems, dma_sems = self.sems, self.dma_sems

        def build(e):
            def body(engine):
                seen = {}
                for o in self.ops[e]:
                    need = {}
                    for d in o.deps:
                        if d.is_dma:
                            k, v = ("d", d.dsem), d.dval
                        else:
                            k, v = ("e", d.eng), d.semv
                        if seen.get(k, 0) >= v:
                            continue
                        if need.get(k, 0) < v:
                            need[k] = v
                    for k, v in need.items():
                        engine.wait_ge(dma_sems[k[1]] if k[0] == "d" else sems[k[1]], v)
                        seen[k] = v
                    if o.fn is None:
                        continue
                    ins = o.fn(engine)
                    if o.is_dma:
                        ins.then_inc(dma_sems[o.dsem], 16)
                    elif o.inc:
                        ins.then_inc(sems[e], 1)
            return body

        block.tensor(build("pe"))
        block.scalar(build("act"))
        block.vector(build("dve"))
        block.gpsimd(build("pool"))
        block.sync(build("sp"))


def build_program(nseq, S, dbg=False):
    NT = S // 128
    NTOK = nseq * S
    nc = bass.Bass("TRN2", target_bir_lowering=False)

    def din(name, shape, dt=F32):
        return nc.dram_tensor(name, list(shape), dt, kind="ExternalInput").ap()

    x = din("x", [NTOK, D])
    w_in = din("w_in", [D, 2560])
    w_out = din("w_out", [D, D])
    w_qT = din("w_qT", [2048, D])
    keysT = din("keysT", [2, 128, 128])
    g_mix = din("g_mix", [128, 8])
    g_mo = din("g_mo", [128, 8])
    g_ffn = din("g_ffn", [128, 8])
    g_q2 = din("g_q2", [128, 1])
    g_k2 = din("g_k2", [128, 1])
    g_vn = din("g_vn", [128, 512])
    w_spT = din("w_spT", [128, 4, 128])
    b_sp = din("b_sp", [128, 4])
    tril = din("tril", [128, 128])
    amask_d = din("amask", [128, 17, 128], BF)
    ident_bf_d = din("ident_bf", [128, 128], BF)
    ident_f_d = din("ident_f", [128, 128])
    iota_bf_d = din("iota_bf", [128, 128], BF)
    iota16_d = din("iota16", [128, 16])
    e_uT = din("e_uT", [NCH, 128, D])
    e_v = din("e_v", [NCH * 128, D])
    out = nc.dram_tensor("out", [NTOK, D], F32, kind="ExternalOutput").ap()
    if dbg:
        dbg_x1 = nc.dram_tensor("dbg_x1", [NTOK, D], F32, kind="ExternalOutput").ap()
    x1s = nc.dram_tensor("x1s", [NTOK, D], F32).ap()
    utb = nc.dram_tensor("utb", [NCH, 128, D], BF).ap()
    vbb = nc.dram_tensor("vbb", [NCH, 128, D], BF).ap()

    S_ = Sched(nc)
    op, dma = S_.op, S_.dma
    dbg_outs = {}

    def dump(name, ap, res, T=0, want=0):
        if not dbg or T != want:
            return
        shp = [int(v) for v in ap.shape]
        t = nc.dram_tensor("dbg_" + name, shp, ap.dtype, kind="ExternalOutput").ap()
        dma(lambda e: e.dma_start(out=t, in_=ap), reads=res if isinstance(res, list) else [res])

    def rsqrt_chain(dst, src, n, reads_r, r_dst):
        op("dve", lambda e: e.tensor_scalar(out=dst, in0=src, scalar1=1.0 / n, scalar2=EPS, op0=ALU.mult, op1=ALU.add),
           reads=reads_r, writes=[r_dst])
        w_ = int(dst.shape[1])
        op("pool", lambda e: e.tensor_tensor(out=dst, in0=dst, in1=mhalf[:, 0:w_], op=ALU.pow), reads=[r_dst, r_c], writes=[r_dst])

    def mm(out_, lhsT, rhs, start, stop, reads, writes):
        op("pe", lambda e: e.matmul(out=out_, lhsT=lhsT, rhs=rhs, start=start, stop=stop), reads, writes)

    def tr(out_, in_, ident, reads, writes):
        op("pe", lambda e: e.transpose(out=out_, in_=in_, identity=ident), reads, writes)

    def actf(out_, in_, func, reads, writes, scale=None):
        if scale is None:
            op("act", lambda e: e.activation(out=out_, in_=in_, func=func), reads, writes)
        else:
            op("act", lambda e: e.activation(out=out_, in_=in_, func=func, scale=scale), reads, writes)

    def tt_(eng, out_, in0, in1, alu, reads, writes):
        op(eng, lambda e: e.tensor_tensor(out=out_, in0=in0, in1=in1, op=alu), reads, writes)

    def ts_(eng, out_, in0, s1, s2, op0, op1, reads, writes):
        if op1 is None:
            op(eng, lambda e: e.tensor_scalar(out=out_, in0=in0, scalar1=s1, scalar2=None, op0=op0), reads, writes)
        else:
            op(eng, lambda e: e.tensor_scalar(out=out_, in0=in0, scalar1=s1, scalar2=s2, op0=op0, op1=op1), reads, writes)

    def red(out_, in_, reads, writes):
        op("dve", lambda e: e.tensor_reduce(out=out_, in_=in_, axis=AX.X, op=ALU.add), reads, writes)

    def cp(eng, out_, in_, reads, writes):
        op(eng, lambda e: e.tensor_copy(out=out_, in_=in_), reads, writes)

    def ld(out_, in_, writes, reads=()):
        dma(lambda e: e.dma_start(out=out_, in_=in_), reads=reads, writes=writes)


    with ExitStack() as top:
        def sb(name, shape, dt, stack=top):
            return stack.enter_context(nc.sbuf_tensor("sb_" + name, list(shape), dt))

        def ps(name, shape, dt, stack):
            return stack.enter_context(nc.psum_tensor("ps_" + name, list(shape), dt))

        ident_bf = sb("ident_bf", [128, 128], BF); r_c = Res()
        ident_f = sb("ident_f", [128, 128], F32)
        iota_bf = sb("iota_bf", [128, 128], BF)
        iota16 = sb("iota16", [128, 16], F32)
        mhalf = sb("mhalf", [128, 16], F32)
        gmix = sb("gmix", [128, 8], F32)
        gmo = sb("gmo", [128, 8], F32)
        gffn = sb("gffn", [128, 8], F32)
        gq2 = sb("gq2", [128, 1], F32)
        gqk = sb("gqk", [128, 1], F32)
        bsp = sb("bsp", [128, 4], F32)
        for dst, src in ((ident_bf, ident_bf_d), (ident_f, ident_f_d), (iota_bf, iota_bf_d), (iota16, iota16_d),
                         (gmix, g_mix), (gmo, g_mo), (gffn, g_ffn), (gq2, g_q2), (gqk, g_k2), (bsp, b_sp)):
            dma(lambda e, dst=dst, src=src: e.dma_start(out=dst[:], in_=src), writes=[r_c])
        op("dve", lambda e: e.tensor_tensor(out=gqk[:], in0=gqk[:], in1=gq2[:], op=ALU.mult), reads=[r_c], writes=[r_c])
        op("pool", lambda e: e.memset(mhalf[:], -0.5), writes=[r_c])

        with ExitStack() as pm:
            w_in_b = sb("w_in_b", [128, 8, 2560], BF, pm); r_win = Res()
            w_out_b = sb("w_out_b", [128, 8, D], BF, pm); r_wout = Res()
            wsT = sb("wsT", [128, 4, 128], BF, pm); r_wsT = Res()
            gvn = sb("gvn", [128, 512], F32, pm)
            amask = sb("amask", [128, 17, 128], BF, pm)
            dma(lambda e: e.dma_start(out=gvn[:], in_=g_vn), writes=[r_c])
            dma(lambda e: e.dma_start(out=amask[:], in_=amask_d), writes=[r_c])
            with ExitStack() as pw:
                wst = [sb(f"wst{i}", [128, 2560], F32, pw) for i in range(2)]
                r_wst = [Res(), Res()]
                trl = sb("trl", [128, 128], F32, pw)
                wspf = sb("wspf", [128, 4, 128], F32, pw)
                dma(lambda e: e.dma_start(out=trl[:], in_=tril), writes=[r_c])
                dma(lambda e: e.dma_start(out=wspf[:], in_=w_spT), writes=[r_c])
                op("dve", lambda e: e.tensor_tensor(out=wsT[:], in0=wspf[:], in1=trl[:].unsqueeze(1).to_broadcast([128, 4, 128]),
                                                    op=ALU.mult), reads=[r_c], writes=[r_wsT])
                for k in range(8):
                    s = k % 2
                    dma(lambda e, k=k, s=s: e.dma_start(out=wst[s][:], in_=w_in[k * 128:(k + 1) * 128, :]), writes=[r_wst[s]])
                    op("act", lambda e, k=k, s=s: e.activation(out=w_in_b[:, k, :], in_=wst[s][:], func=AF.Identity,
                                                               scale=gmix[:, k:k + 1]), reads=[r_wst[s], r_c], writes=[r_win])
                for k in range(8):
                    s = k % 2
                    dma(lambda e, k=k, s=s: e.dma_start(out=wst[s][:, 0:D], in_=w_out[k * 128:(k + 1) * 128, :]), writes=[r_wst[s]])
                    op("act", lambda e, k=k, s=s: e.activation(out=w_out_b[:, k, :], in_=wst[s][:, 0:D], func=AF.Identity,
                                                               scale=gmo[:, k:k + 1]), reads=[r_wst[s], r_c], writes=[r_wout])
                S_.barrier()

            xt = [sb(f"xt{i}", [128, D], F32, pm) for i in range(3)]; r_xt = [Res(), Res(), Res()]
            ssx = sb("ssx", [128, 1], F32, pm); r_ssx = Res()
            ss2 = sb("ss2", [128, 2], F32, pm); r_ss2 = Res()
            rstd = sb("rstd", [128, 1], F32, pm); r_rstd = Res()
            xb = sb("xb", [128, D], BF, pm); r_xb = Res()
            xT = sb("xT", [128, 8, 128], BF, pm); r_xT = Res()
            sq = sb("sq", [128, D], F32, pm); r_sq = Res()
            sq2 = sb("sq2", [128, 512], F32, pm); r_sq2 = Res()
            ss16 = sb("ss16", [128, 16], F32, pm); r_ss16 = Res()
            rs16 = sb("rs16", [128, 16], F32, pm); r_rs16 = Res()
            qkn = sb("qkn", [128, D], BF, pm); r_qkn = Res()
            qT = [sb(f"qT{i}", [128, 4, 128], BF, pm) for i in range(2)]; r_qT = [Res(), Res()]
            kT = sb("kT", [128, 4, S], BF, pm); r_kT = [Res() for _ in range(NT)]
            Va = sb("Va", [128, NT, 8, 65], BF, pm); r_V = [Res() for _ in range(NT)]
            ug = [sb(f"ug{i}", [128, 512], F32, pm) for i in range(2)]; r_ug = [Res(), Res()]
            vg = sb("vg", [128, 512], F32, pm); r_vg = Res()
            ss4 = sb("ss4", [128, 4], F32, pm); r_ss4 = Res()
            rs4 = sb("rs4", [128, 4], F32, pm); r_rs4 = Res()
            vgs = sb("vgs", [128, 512], F32, pm); r_vgs = Res()
            vgn = [sb(f"vgn{i}", [128, 512], BF, pm) for i in range(2)]; r_vgn = [Res(), Res()]
            gated = sb("gated", [128, 512], F32, pm); r_gated = Res()
            ssg = sb("ssg", [128, 1], F32, pm); r_ssg = Res()
            rsg = sb("rsg", [128, 1], F32, pm); r_rsg = Res()
            NP = 5
            Pb = sb("Pb", [128, NP, 4, 128], BF, pm); r_P = [Res() for _ in range(NP)]
            rden = sb("rden", [128, 8], F32, pm); r_rden = Res()
            attf = sb("attf", [128, 512], F32, pm); r_attf = Res()
            ssa = sb("ssa", [128, 1], F32, pm); r_ssa = Res()
            rsa = sb("rsa", [128, 1], F32, pm); r_rsa = Res()
            merged = sb("merged", [128, D], BF, pm); r_mg = Res()
            mT = sb("mT", [128, 8, 128], BF, pm); r_mT = Res()
            x1 = [sb(f"x1_{i}", [128, D], F32, pm) for i in range(2)]; r_x1 = [Res(), Res()]

            psT = ps("psT", [128, 8, 128], BF, pm); r_psT = Res()
            NSS = 3
            psS = ps("psS", [128, NSS, 4, 128], F32, pm); r_psS = [Res() for _ in range(NSS)]
            psOt = ps("psO", [128, 8, 128], F32, pm); r_pO = [Res(), Res()]
            psO = psOt[:]
            Fp = ps("Fp", [128, 1024], F32, pm); r_F = [Res(), Res()]
            psZ = Fp[:, 0:512].rearrange("p (g c) -> p g c", c=128)

            ust = [sb(f"ust{i}", [128, D], F32, pm) for i in range(2)]; vst = [sb(f"vst{i}", [128, D], F32, pm) for i in range(2)]
            ubs = [sb("ubs0", [128, D], BF, pm)] * 2; vbs = [sb("vbs0", [128, D], BF, pm)] * 2
            r_ust = [Res(), Res()]; r_vst = [Res(), Res()]; r_ubs = [Res()] * 2; r_vbs = [Res()] * 2

            def tab_load(g):
                sl_ = g % 2
                ld(ust[sl_][:], e_uT[g], [r_ust[sl_]])
                ld(vst[sl_][:], e_v[128 * g:128 * g + 128, :], [r_vst[sl_]])

            def tab_conv(g):
                sl_ = g % 2
                for hh in range(2):
                    tt_("dve", ubs[sl_][:, hh * 512:(hh + 1) * 512].rearrange("p (k e) -> p k e", e=128),
                        ust[sl_][:, hh * 512:(hh + 1) * 512].rearrange("p (k e) -> p k e", e=128),
                        gffn[:, hh * 4:(hh + 1) * 4].unsqueeze(2).to_broadcast([128, 4, 128]), ALU.mult, [r_ust[sl_], r_c], [r_ubs[sl_]])
                cp("pool", vbs[sl_][:], vst[sl_][:], [r_vst[sl_]], [r_vbs[sl_]])
                ld(utb[g], ubs[sl_][:], [], reads=[r_ubs[sl_]])
                ld(vbb[g], vbs[sl_][:], [], reads=[r_vbs[sl_]])

            op("pool", lambda e: e.memset(Va[:, :, :, 64:65], 1.0), writes=r_V)

            tiles = [(s, i) for s in range(nseq) for i in range(NT)]

            def front_gen(T):
                s, i = tiles[T]
                r0 = s * S + i * 128
                xs = xt[T % 3]; rx = r_xt[T % 3]
                sl = T % 2
                ld(xs[:], x[r0:r0 + 128, :], [rx])
                yield
                for hh in range(2):
                    tt_("dve", sq[:, hh * 512:(hh + 1) * 512], xs[:, hh * 512:(hh + 1) * 512], xs[:, hh * 512:(hh + 1) * 512], ALU.mult,
                        [rx], [r_sq])
                    yield
                    cp("dve", xb[:, hh * 512:(hh + 1) * 512], xs[:, hh * 512:(hh + 1) * 512], [rx], [r_xb])
                    yield
                for hh in range(2):
                    red(ss2[:, hh:hh + 1], sq[:, hh * 512:(hh + 1) * 512], [r_sq], [r_ss2])
                    yield
                red(ssx[:], ss2[:], [r_ss2], [r_ssx])
                rsqrt_chain(rstd[:], ssx[:], D, [r_ssx], r_rstd)
                for k in range(8):
                    tr(psT[:, k, :], xb[:, k * 128:(k + 1) * 128], ident_bf[:], [r_xb, r_c], [r_psT])
                yield
                for hh in range(2):
                    cp("dve", xT[:, hh * 4:(hh + 1) * 4, :], psT[:, hh * 4:(hh + 1) * 4, :], [r_psT], [r_xT])
                    yield
                for n in range(2):
                    for k in range(8):
                        mm(Fp[:, n * 512:(n + 1) * 512], xT[:, k, :], w_in_b[:, k, n * 512:(n + 1) * 512], k == 0, k == 7,
                           [r_xT, r_win], [r_F[n]])
                    yield
                actf(sq[:], Fp[:, 0:1024], AF.Square, [r_F[0], r_F[1], r_rstd], [r_sq], scale=rstd[:])
                yield
                for hh in range(2):
                    red(ss16[:, hh * 8:(hh + 1) * 8], sq[:, hh * 512:(hh + 1) * 512].rearrange("p (h c) -> p h c", c=64), [r_sq], [r_ss16])
                    yield
                rsqrt_chain(rs16[:], ss16[:], 64, [r_ss16], r_rs16)
                ts_("dve", rs16[:], rs16[:], rstd[:], None, ALU.mult, None, [r_rs16, r_rstd], [r_rs16])
                yield
                for hh in range(2):
                    tt_("dve", qkn[:, hh * 512:(hh + 1) * 512].rearrange("p (h c) -> p h c", c=64),
                        Fp[:, hh * 512:(hh + 1) * 512].rearrange("p (h c) -> p h c", c=64),
                        rs16[:, hh * 8:(hh + 1) * 8].unsqueeze(2).to_broadcast([128, 8, 64]), ALU.mult, [r_F[hh], r_rs16], [r_qkn])
                    yield
                for n in range(2):
                    for k in range(8):
                        mm(Fp[:, n * 512:(n + 1) * 512], xT[:, k, :], w_in_b[:, k, (2 + n) * 512:(3 + n) * 512], k == 0, k == 7,
                           [r_xT, r_win], [r_F[n]])
                    yield
                ts_("dve", Va[:, i, :, 0:64], Fp[:, 0:512].rearrange("p (h c) -> p h c", c=64), rstd[:], None, ALU.mult, None,
                    [r_F[0], r_rstd], [r_V[i]])
                actf(ug[sl][:], Fp[:, 512:1024], AF.Gelu, [r_F[1], r_rstd], [r_ug[sl]], scale=rstd[:])
                yield
                for k in range(8):
                    mm(Fp[:, 0:512], xT[:, k, :], w_in_b[:, k, 2048:2560], k == 0, k == 7, [r_xT, r_win], [r_F[0]])
                yield
                for k in range(8):
                    tr(psT[:, k, :], qkn[:, k * 128:(k + 1) * 128], ident_bf[:], [r_qkn, r_c], [r_psT])
                yield
                cp("dve", qT[sl][:], psT[:, 0:4, :], [r_psT], [r_qT[sl]])
                yield
                ts_("dve", kT[:, :, i * 128:(i + 1) * 128], psT[:, 4:8, :], gqk[:], None, ALU.mult, None, [r_psT, r_c], [r_kT[i]])
                yield
                actf(vg[:], Fp[:, 0:512], AF.Gelu, [r_F[0], r_rstd], [r_vg], scale=rstd[:])
                yield
                tt_("dve", sq[:, 0:512], vg[:], vg[:], ALU.mult, [r_vg], [r_sq])
                yield
                red(ss4[:], sq[:, 0:512].rearrange("p (g c) -> p g c", c=128), [r_sq], [r_ss4])
                rsqrt_chain(rs4[:], ss4[:], 128, [r_ss4], r_rs4)
                yield
                tt_("dve", vgs[:].rearrange("p (g c) -> p g c", c=128), vg[:].rearrange("p (g c) -> p g c", c=128),
                    rs4[:].unsqueeze(2).to_broadcast([128, 4, 128]), ALU.mult, [r_vg, r_rs4], [r_vgs])
                yield
                tt_("pool", vgn[sl][:], vgs[:], gvn[:], ALU.mult, [r_vgs, r_c], [r_vgn[sl]])
                yield

            cnt = {"ps": 0, "p": 0}

            def attn(T, filler, n_f):
                s, i = tiles[T]
                r0 = s * S + i * 128
                sl = T % 2
                lo = max(0, i - 16)
                kts = list(range(lo, i + 1))
                items = [(h, kts[a:a + 4]) for h in range(8) for a in range(0, len(kts), 4)]

                def emit_st(it):
                    h, batch = it
                    ssl = cnt["ps"] % NSS; cnt["ps"] += 1
                    pl = cnt["p"] % NP; cnt["p"] += 1
                    pr, hp = h // 2, h % 2
                    for b_, j in enumerate(batch):
                        mm(psS[:, ssl, b_, :], kT[hp * 64:(hp + 1) * 64, pr, j * 128:(j + 1) * 128], qT[sl][hp * 64:(hp + 1) * 64, pr, :],
                           True, True, [r_kT[j], r_qT[sl]], [r_psS[ssl]])
                    nb = len(batch)
                    op("act", lambda e, ssl=ssl, pl=pl, nb=nb: e.activation(out=Pb[:, pl, 0:nb, :], in_=psS[:, ssl, 0:nb, :],
                                                                            func=AF.Exp, scale=0.125), [r_psS[ssl]], [r_P[pl]])
                    idx0 = 16 - (i - batch[0])
                    tt_("dve", Pb[:, pl, 0:nb, :], Pb[:, pl, 0:nb, :], amask[:, idx0:idx0 + nb, :], ALU.mult, [r_P[pl], r_c], [r_P[pl]])
                    return pl

                def emit_pv(it, pl):
                    h, batch = it
                    for b_, j in enumerate(batch):
                        mm(psO[:, h, 0:65], Pb[:, pl, b_, :], Va[:, j, h, :], j == kts[0], j == kts[-1],
                           [r_P[pl], r_V[j]], [r_pO[h // 4]])

                gpt = -(-NCH // len(tiles))
                my_groups = [g for g in range(T * gpt, min(NCH, (T + 1) * gpt))]
                for g in my_groups[:2]:
                    tab_load(g)
                pace = n_f / max(1, len(items))
                acc = 0.0
                pls = [emit_st(it_) for it_ in items[:2]]
                for n in range(len(items)):
                    if n + 2 < len(items):
                        pls.append(emit_st(items[n + 2]))
                    emit_pv(items[n], pls[n])
                    acc += pace
                    while filler is not None and acc >= 1.0:
                        acc -= 1.0
                        if next(filler, "done") == "done":
                            filler = None
                if filler is not None:
                    for _ in filler:
                        pass
                op("dve", lambda e: e.reciprocal(out=rden[:], in_=psO[:, :, 64]), [r_pO[0], r_pO[1]], [r_rden])
                tt_("dve", attf[:].rearrange("p (h c) -> p h c", c=64), psO[:, :, 0:64],
                    rden[:].unsqueeze(2).to_broadcast([128, 8, 64]), ALU.mult, [r_pO[0], r_pO[1], r_rden], [r_attf])
                for n_, g in enumerate(my_groups):
                    tab_conv(g)
                    if n_ + 2 < len(my_groups):
                        tab_load(my_groups[n_ + 2])

            def tail_gen(T):
                s, i = tiles[T]
                r0 = s * S + i * 128
                xs = xt[T % 3]; rx = r_xt[T % 3]
                sl = T % 2
                tt_("dve", sq2[:], attf[:], attf[:], ALU.mult, [r_attf], [r_sq2])
                yield
                red(ssa[:], sq2[:], [r_sq2], [r_ssa])
                rsqrt_chain(rsa[:], ssa[:], 512, [r_ssa], r_rsa)
                yield
                ts_("dve", merged[:, 0:512], attf[:], rsa[:], None, ALU.mult, None, [r_attf, r_rsa], [r_mg])
                yield
                for g in range(4):
                    mm(psZ[:, g, :], wsT[:, g, :], vgn[sl][:, g * 128:(g + 1) * 128], True, True, [r_wsT, r_vgn[sl]], [r_F[0]])
                for g in range(4):
                    op("dve", lambda e, g=g, sl=sl: e.scalar_tensor_tensor(out=gated[:, g * 128:(g + 1) * 128], in0=psZ[:, g, :],
                                                                           scalar=bsp[:, g:g + 1], in1=ug[sl][:, g * 128:(g + 1) * 128],
                                                                           op0=ALU.add, op1=ALU.mult),
                       [r_F[0], r_ug[sl], r_c], [r_gated])
                yield
                tt_("dve", sq2[:], gated[:], gated[:], ALU.mult, [r_gated], [r_sq2])
                yield
                red(ssg[:], sq2[:], [r_sq2], [r_ssg])
                rsqrt_chain(rsg[:], ssg[:], 512, [r_ssg], r_rsg)
                yield
                ts_("dve", merged[:, 512:1024], gated[:], rsg[:], None, ALU.mult, None, [r_gated, r_rsg], [r_mg])
                yield
                for k in range(8):
                    tr(psT[:, k, :], merged[:, k * 128:(k + 1) * 128], ident_bf[:], [r_mg, r_c], [r_psT])
                yield
                for hh in range(2):
                    cp("dve", mT[:, hh * 4:(hh + 1) * 4, :], psT[:, hh * 4:(hh + 1) * 4, :], [r_psT], [r_mT])
                    yield
                xo = x1[T % 2]; rxo = r_x1[T % 2]
                for n in range(2):
                    bk = 1 - n
                    for k in range(8):
                        mm(Fp[:, bk * 512:(bk + 1) * 512], mT[:, k, :], w_out_b[:, k, n * 512:(n + 1) * 512], k == 0, k == 7,
                           [r_mT, r_wout], [r_F[bk]])
                    yield
                    tt_("dve", xo[:, n * 512:(n + 1) * 512], Fp[:, bk * 512:(bk + 1) * 512], xs[:, n * 512:(n + 1) * 512], ALU.add,
                        [r_F[bk], rx], [rxo])
                ld(x1s[r0:r0 + 128, :], xo[:], [], reads=[rxo])
                if dbg:
                    ld(dbg_x1[r0:r0 + 128, :], xo[:], [], reads=[rxo])
                yield

            for _ in front_gen(0):
                pass

            def chain(*gens):
                for g_ in gens:
                    if g_ is not None:
                        for v_ in g_:
                            yield v_

            nt_ = len(tiles)
            for T in range(nt_):
                tg = tail_gen(T - 1) if T > 0 else None
                fg = front_gen(T + 1) if T + 1 < nt_ else None
                n_f = (14.0 if T > 0 else 0.0) + (34.0 if fg is not None else 0.0)
                if fg is not None:
                    next(fg)
                attn(T, chain(tg, fg), n_f)
            for _ in tail_gen(nt_ - 1):
                pass
            S_.barrier()

        with ExitStack() as pp:
            WS = sb("WS", [128, 8, 2048], BF, pp); r_WS = Res()
            with ExitStack() as pw:
                kTt = sb("kTt", [128, 2, 128], F32, pw)
                wq = [sb(f"wq{i}", [128, D], F32, pw) for i in range(2)]; r_wq = [Res(), Res()]
                psW0 = ps("psW0", [128, 8, 128], F32, pw); r_psW0 = Res()
                dma(lambda e: e.dma_start(out=kTt[:], in_=keysT.rearrange("s c k -> c s k")), writes=[r_c])
                for hs in range(16):
                    s = hs % 2
                    dma(lambda e, hs=hs, s=s: e.dma_start(out=wq[s][:], in_=w_qT[hs * 128:(hs + 1) * 128, :]), writes=[r_wq[s]])
                    for k in range(8):
                        op("pe", lambda e, hs=hs, s=s, k=k: e.matmul(out=psW0[:, k, :], lhsT=wq[s][:, k * 128:(k + 1) * 128],
                                                                     rhs=kTt[:, hs % 2, :], start=True, stop=True),
                           reads=[r_wq[s], r_c], writes=[r_psW0])
                    op("dve", lambda e, hs=hs: e.tensor_tensor(out=WS[:, :, hs * 128:(hs + 1) * 128], in0=psW0[:],
                                                               in1=gffn[:].unsqueeze(2).to_broadcast([128, 8, 128]), op=ALU.mult),
                       reads=[r_psW0, r_c], writes=[r_WS])
                S_.barrier()

            TB = 256
            NB = NTOK // TB
            GC = 2
            NG = NCH // GC

            x1p = [sb("x1p0", [128, D], F32, pp)] * 2; r_x1p = [Res()] * 2
            x1r = sb("x1r", [128, D], F32, pp); r_x1r = Res()
            ssxp = sb("ssxp", [128, 1], F32, pp); r_ssxp = Res()
            rstdp = sb("rstdp", [128, 1], F32, pp); r_rstdp = Res()
            xnb = sb("xnb", [128, D], BF, pp); r_xnb = Res()
            XnT = [sb(f"XnT{i}", [128, 8, TB], BF, pp) for i in range(2)]; r_XnT = [Res(), Res()]
            S1 = sb("S1", [128, 2048], F32, pp); r_S1 = Res()
            S2 = sb("S2", [128, 2048], F32, pp); r_S2 = Res()
            S3 = S1; r_S3 = r_S1
            vab = sb("vab", [128, 8, 2, 16], F32, pp); r_vab = [Res() for _ in range(16)]
            iabu = sb("iabu", [128, 8, 2, 16], U32, pp); r_iabu = [Res() for _ in range(16)]
            r_S2v = [Res() for _ in range(16)]; r_S2c = [Res() for _ in range(8)]
            r_topsh = [Res() for _ in range(8)]; r_posuh = [Res() for _ in range(8)]
            iabf = sb("iabf", [128, 8, 2, 16], F32, pp); r_iabf = Res()
            tops = sb("tops", [128, 8, 16], F32, pp); r_tops = Res()
            posu = sb("posu", [128, 8, 16], U32, pp); r_posu = Res()
            pij_u = sb("pij_u", [128, 2, 128], U32, pp); r_piju = Res()
            pij_f = sb("pij_f", [128, 2, 128], F32, pp); r_pijf = Res()
            sel = sb("sel", [128, 3, 128], F32, pp); r_sel = Res()
            sm8 = sb("sm8", [128, 8], F32, pp); r_sm8 = Res()
            pickT = sb("pickT", [128, 3, 128], BF, pp); r_pickT = Res()
            pickF = sb("pickF", [128, 3, 128], F32, pp)
            TG = 8
            iota3 = sb("iota3", [128, 128, TG], BF, pp)
            Ag = [sb(f"Ag{i}", [128, TG, 128], BF, pp) for i in range(2)]
            Bg = [sb(f"Bg{i}", [128, 128, TG], BF, pp) for i in range(2)]
            r_AB = [Res(), Res()]
            op("dve", lambda e: e.tensor_copy(out=iota3[:], in_=iota_bf[:].unsqueeze(2).to_broadcast([128, 128, TG])),
               reads=[r_c], writes=[r_c])
            WTb = [sb(f"WTb{i}", [128, 128, 128], BF, pp) for i in range(3)]; r_WTb = [Res() for _ in range(3)]
            Gt = sb("Gt", [128, 2, TB], BF, pp); r_G = [Res(), Res()]
            Ct = sb("Ct", [128, 2, TB], BF, pp); r_C = [[Res(), Res()], [Res(), Res()]]
            Ug = [sb(f"Ug{i}", [128, GC, D], BF, pp) for i in range(2)]; r_Ug = [Res(), Res()]
            Vg = [sb(f"Vg{i}", [128, GC, D], BF, pp) for i in range(2)]; r_Vg = [Res(), Res()]

            psF = ps("psF", [128, 512], F32, pp); r_psF = Res()
            psTq = psF[:].bitcast(BF).rearrange("p (k t) -> p k t", t=128); r_psTq = r_psF
            psWa = ps("psWa", [128, 512], F32, pp); r_psWa = Res()
            psWs = [psWa[:].rearrange("p (t a) -> p t a", a=128), psF[:].rearrange("p (t a) -> p t a", a=128)]
            r_psW = [r_psWa, r_psF]
            big = ps("big", [128, 4, 512], F32, pp); r_big = [Res() for _ in range(4)]
            psH = ps("psH", [128, 2, 512], F32, pp); r_psH = [Res(), Res()]

            S2v = S2[:].rearrange("p (a b) -> p a b", b=128)
            S2c = S2[:].rearrange("p (a b) -> p a b", b=256)
            S2e = S2[:].rearrange("p (a b) -> p a b", b=16)
            S3c = S3[:].rearrange("p (a b) -> p a b", b=256)

            def prep_gen(b):
                xsl = b % 2
                for tt in range(2):
                    gt = 2 * b + tt
                    wsl = gt % 3
                    xs = x1p[gt % 2]; rx = r_x1p[gt % 2]
                    r0 = b * TB + tt * 128
                    ld(xs[:], x1s[r0:r0 + 128, :], [rx])
                    yield 0.5
                    yield 0.5
                    actf(S3[:, 0:D], xs[:], AF.Square, [rx], [r_S3])
                    yield 1.0
                    red(ssxp[:], S3[:, 0:D], [r_S3], [r_ssxp])
                    rsqrt_chain(rstdp[:], ssxp[:], D, [r_ssxp], r_rstdp)
                    yield 1.2
                    yield 0.8
                    actf(xnb[:], xs[:], AF.Identity, [rx, r_rstdp], [r_xnb], scale=rstdp[:])
                    yield 1.0
                    for k in range(8):
                        tr(psTq[:, k, :], xnb[:, k * 128:(k + 1) * 128], ident_bf[:], [r_xnb, r_c], [r_psTq])
                    yield 0.8
                    actf(XnT[xsl][:, :, tt * 128:(tt + 1) * 128], psTq, AF.Copy, [r_psTq], [r_XnT[xsl]])
                    yield 0.9
                    for n in range(4):
                        for k in range(8):
                            mm(psF[:], XnT[xsl][:, k, tt * 128:(tt + 1) * 128], WS[:, k, n * 512:(n + 1) * 512],
                               k == 0, k == 7, [r_XnT[xsl], r_WS], [r_psF])
                        yield 1.9
                        actf(S1[:, n * 512:(n + 1) * 512], psF[:], AF.Copy, [r_psF], [r_S1])
                        yield 0.8
                    for step in range(5):
                        for hs in range(16):
                            src = S1[:, hs * 128:(hs + 1) * 128]
                            m = vab[:, hs // 2, hs % 2, :]; ix = iabu[:, hs // 2, hs % 2, :]
                            v2 = S2v[:, hs, :]
                            rv_, ri_, r2_ = r_vab[hs], r_iabu[hs], r_S2v[hs]
                            if step == 0:
                                op("dve", lambda e, m=m, src=src: e.max(out=m[:, 0:8], in_=src), [r_S1], [rv_])
                            elif step == 1:
                                op("dve", lambda e, m=m, ix=ix, src=src: e.max_index(out=ix[:, 0:8], in_max=m[:, 0:8], in_values=src),
                                   [r_S1, rv_], [ri_])
                            elif step == 2:
                                op("dve", lambda e, m=m, src=src, v2=v2: e.match_replace(out=v2, in_to_replace=m[:, 0:8], in_values=src,
                                                                                         imm_value=-1e30), [r_S1, rv_], [r2_, r_S2c[hs // 2]])
                            elif step == 3:
                                op("dve", lambda e, m=m, v2=v2: e.max(out=m[:, 8:16], in_=v2), [r2_], [rv_])
                            else:
                                op("dve", lambda e, m=m, ix=ix, v2=v2: e.max_index(out=ix[:, 8:16], in_max=m[:, 8:16], in_values=v2),
                                   [r2_, rv_], [ri_])
                            if hs % 4 == 3:
                                yield 1.0
                    for hp in range(4):
                        tt_("dve", S3[:, hp * 512:(hp + 1) * 512].rearrange("p (h a b) -> p h a b", a=16, b=16),
                            vab[:, 2 * hp:2 * hp + 2, 0, :].unsqueeze(3).to_broadcast([128, 2, 16, 16]),
                            vab[:, 2 * hp:2 * hp + 2, 1, :].unsqueeze(2).to_broadcast([128, 2, 16, 16]), ALU.add, r_vab, [r_S3])
                        yield 0.6
                    for step in range(5):
                        for h in range(8):
                            cf = S3c[:, h, :]; v2 = S2c[:, h, :]
                            tp = tops[:, h, :]; px = posu[:, h, :]
                            rt_, rp_, r2_ = r_topsh[h], r_posuh[h], r_S2c[h]
                            if step == 0:
                                op("dve", lambda e, tp=tp, cf=cf: e.max(out=tp[:, 0:8], in_=cf), [r_S3], [rt_])
                            elif step == 1:
                                op("dve", lambda e, tp=tp, px=px, cf=cf: e.max_index(out=px[:, 0:8], in_max=tp[:, 0:8], in_values=cf),
                                   [r_S3, rt_], [rp_])
                            elif step == 2:
                                op("dve", lambda e, tp=tp, cf=cf, v2=v2: e.match_replace(out=v2, in_to_replace=tp[:, 0:8], in_values=cf,
                                                                                         imm_value=-1e30),
                                   [r_S3, rt_], [r2_, r_S2v[2 * h], r_S2v[2 * h + 1]])
                            elif step == 3:
                                op("dve", lambda e, tp=tp, v2=v2: e.max(out=tp[:, 8:16], in_=v2), [r2_], [rt_])
                            else:
                                op("dve", lambda e, tp=tp, px=px, v2=v2: e.max_index(out=px[:, 8:16], in_max=tp[:, 8:16], in_values=v2),
                                   [r2_, rt_], [rp_])
                            if h % 4 == 3:
                                yield 1.4
                    pf = posu[:].rearrange("p h k -> p (h k)")
                    op("dve", lambda e, pf=pf: e.tensor_single_scalar(out=pij_u[:, 0, :], in_=pf, scalar=4, op=ALU.logical_shift_right),
                       r_posuh, [r_piju])
                    op("dve", lambda e, pf=pf: e.tensor_single_scalar(out=pij_u[:, 1, :], in_=pf, scalar=15, op=ALU.bitwise_and),
                       r_posuh, [r_piju])
                    cp("dve", pij_f[:], pij_u[:], [r_piju], [r_pijf])
                    cp("dve", iabf[:], iabu[:], r_iabu, [r_iabf])
                    yield 0.9
                    for sd in range(2):
                        for hp in range(4):
                            e3 = S2[:, hp * 512:(hp + 1) * 512].rearrange("p (k i) -> p k i", i=16)
                            e4 = S2[:, hp * 512:(hp + 1) * 512].rearrange("p (h k i) -> p h k i", k=16, i=16)
                            tt_("dve", e3, iota16[:].unsqueeze(1).to_broadcast([128, 32, 16]),
                                pij_f[:, sd, hp * 32:(hp + 1) * 32].unsqueeze(2).to_broadcast([128, 32, 16]), ALU.is_equal,
                                [r_pijf, r_c], [r_S2] + r_S2v + r_S2c)
                            tt_("dve", e4, e4, iabf[:, 2 * hp:2 * hp + 2, sd, :].unsqueeze(2).to_broadcast([128, 2, 16, 16]), ALU.mult,
                                [r_S2, r_iabf], [r_S2] + r_S2v + r_S2c)
                            yield 1.2
                            red(sel[:, sd, hp * 32:(hp + 1) * 32], e3, [r_S2] + r_S2v + r_S2c, [r_sel])
                            yield 0.6
                    g3 = sel[:, 2, :].rearrange("p (h k) -> p h k", k=16)
                    tt_("dve", g3, tops[:], tops[:, :, 0:1].to_broadcast([128, 8, 16]), ALU.subtract, r_topsh, [r_sel])
                    yield 0.5
                    actf(sel[:, 2, :], sel[:, 2, :], AF.Exp, [r_sel], [r_sel])
                    yield 0.5
                    red(sm8[:], g3, [r_sel], [r_sm8])
                    op("dve", lambda e: e.reciprocal(out=sm8[:], in_=sm8[:]), [r_sm8], [r_sm8])
                    tt_("dve", g3, g3, sm8[:].unsqueeze(2).to_broadcast([128, 8, 16]), ALU.mult, [r_sel, r_sm8], [r_sel])
                    yield 0.8
                    for q in range(3):
                        tr(psF[:, q * 128:(q + 1) * 128], sel[:, q, :], ident_f[:], [r_sel, r_c], [r_psF])
                    yield 0.5
                    actf(pickT[:].rearrange("p q t -> p (q t)"), psF[:, 0:384], AF.Copy, [r_psF], [r_pickT])
                    actf(pickF[:].rearrange("p q t -> p (q t)"), psF[:, 0:384], AF.Copy, [r_psF], [r_pickT])
                    yield 1.0
                    NGR = 128 // TG
                    if tt == 1:
                        yield "barrier"

                    fastA = (tt == 1)

                    def onehot_b(g):
                        sl = g % 2
                        t0 = g * TG
                        tt_("dve", Bg[sl][:], iota3[:], pickT[:, 1, t0:t0 + TG].unsqueeze(1).to_broadcast([128, 128, TG]),
                            ALU.is_equal, [r_pickT, r_c], [r_AB[sl]])
                        if fastA:
                            a3 = Ag[sl][:].rearrange("p t a -> p (t a)").rearrange("p (a t) -> p a t", t=TG)
                            tt_("dve", a3, iota3[:], pickT[:, 0, t0:t0 + TG].unsqueeze(1).to_broadcast([128, 128, TG]),
                                ALU.is_equal, [r_pickT, r_c], [r_AB[sl]])
                            tt_("dve", a3, a3, pickT[:, 2, t0:t0 + TG].unsqueeze(1).to_broadcast([128, 128, TG]),
                                ALU.mult, [r_pickT, r_AB[sl]], [r_AB[sl]])

                    def onehot_a(g, half):
                        if fastA:
                            return
                        sl = g % 2
                        t0 = g * TG
                        for u in range(half * 4, half * 4 + 4):
                            ts_("dve", Ag[sl][:, u, :], iota_bf[:], pickF[:, 0, t0 + u:t0 + u + 1], pickF[:, 2, t0 + u:t0 + u + 1],
                                ALU.is_equal, ALU.mult, [r_pickT, r_c], [r_AB[sl]])

                    def wmm(q):
                        g = (4 * q) // TG
                        sl = g % 2
                        for u in range(4):
                            ti = (4 * q + u) % TG
                            if fastA:
                                rhs = Ag[sl][:].rearrange("p t a -> p (t a)").rearrange("p (a t) -> p a t", t=TG)[:, :, ti]
                            else:
                                rhs = Ag[sl][:, ti, :]
                            mm(psWs[q % 2][:, u, :], Bg[sl][:, :, ti], rhs, True, True, [r_AB[sl]], [r_psW[q % 2]])

                    def wev(q):
                        actf(WTb[wsl][:, 4 * q:4 * q + 4, :], psWs[q % 2], AF.Copy, [r_psW[q % 2]], [r_WTb[wsl]])

                    NQ = 32
                    for g in range(NGR + 2):
                        if g < NGR:
                            onehot_b(g)
                            onehot_a(g, 0)
                        if 0 <= 2 * g - 2 < NQ:
                            wmm(2 * g - 2)
                        if 0 <= 2 * g - 3 < NQ:
                            wev(2 * g - 3)
                        yield 1.7
                        if g < NGR:
                            onehot_a(g, 1)
                        if 0 <= 2 * g - 1 < NQ:
                            wmm(2 * g - 1)
                        if 0 <= 2 * g - 2 < NQ:
                            wev(2 * g - 2)
                        yield 1.1

            def load_tables(gidx):
                s = gidx % 2
                g = gidx % NG
                ld(Ug[s][:], utb[g * GC:(g + 1) * GC].rearrange("c p f -> p c f"), [r_Ug[s]])
                ld(Vg[s][:], vbb[g * GC:(g + 1) * GC].rearrange("c p f -> p c f"), [r_Vg[s]])

            def emit_h(b, c, gslot):
                hs = c % 2
                cc = c % GC
                xsl = b % 2
                for k in range(8):
                    mm(psH[:, hs, 0:TB], Ug[gslot][:, cc, k * 128:(k + 1) * 128], XnT[xsl][:, k, :], k == 0, k == 7,
                       [r_Ug[gslot], r_XnT[xsl]], [r_psH[hs]])
                actf(Gt[:, hs, :], psH[:, hs, 0:TB], AF.Gelu, [r_psH[hs]], [r_G[hs]])
                for t2 in range(2):
                    wsl = (2 * b + t2) % 3
                    tt_(C_ENG[t2], Ct[:, hs, t2 * 128:(t2 + 1) * 128], Gt[:, hs, t2 * 128:(t2 + 1) * 128], WTb[wsl][:, :, c],
                        ALU.mult, [r_G[hs], r_WTb[wsl]], [r_C[hs][t2]])

            def emit_o(c, gslot):
                hs = c % 2
                cc = c % GC
                for t2 in range(2):
                    for nh in range(2):
                        mm(big[:, t2 * 2 + nh, :], Ct[:, hs, t2 * 128:(t2 + 1) * 128], Vg[gslot][:, cc, nh * 512:(nh + 1) * 512],
                           c == 0, c == NCH - 1, [r_C[hs][t2], r_Vg[gslot]], [r_big[t2 * 2 + nh]])

            C_ENG = ("pool", "pool")

            S_.dry = True
            n_units = 0.0
            for r_ in prep_gen(1):
                if r_ == "barrier":
                    break
                n_units += r_
            S_.dry = False
            PACE = n_units / float(NCH - 6)

            for _ in prep_gen(0):
                pass
            gcount = 0
            load_tables(0)
            for b in range(NB):
                filler = prep_gen(b + 1) if b + 1 < NB else None
                stalled = False
                acc = 0.0
                slots = {}
                for c in range(NCH):
                    if c % GC == 0:
                        slots[c // GC] = gcount % 2
                        gcount += 1
                    emit_h(b, c, slots[c // GC])
                    if c >= 1:
                        emit_o(c - 1, slots[(c - 1) // GC])
                    if c % GC == 0 and not (b == NB - 1 and c // GC == NG - 1):
                        load_tables(gcount)
                    acc += PACE
                    while filler is not None and not stalled and acc > 0.0:
                        r_ = next(filler, "done")
                        if r_ == "done":
                            filler = None
                        elif r_ == "barrier":
                            stalled = True
                        else:
                            acc -= r_
                emit_o(NCH - 1, slots[(NCH - 1) // GC])
                if filler is not None:
                    for _ in filler:
                        pass
                for t2 in range(2):
                    r0 = b * TB + t2 * 128
                    ld(x1r[:], x1s[r0:r0 + 128, :], [r_x1r])
                    tt_("dve", x1r[:], big[:, 2 * t2:2 * t2 + 2, :].rearrange("p n f -> p (n f)"), x1r[:], ALU.add,
                        [r_big[2 * t2], r_big[2 * t2 + 1], r_x1r], [r_x1r])
                    dma(lambda e, r0=r0: e.dma_start(out=out[r0:r0 + 128, :], in_=x1r[:]), reads=[r_x1r])
            S_.barrier()

        with nc.Block() as block:
            S_.emit(block)
    return nc


def _attn_mask():
    k = np.arange(128)[:, None, None]
    idx = np.arange(17)[None, :, None]
    q = np.arange(128)[None, None, :]
    dist = (16 - idx) * 128 + q - k
    m = ((dist >= 0) & (dist <= 128)).astype(np.float32)
    m += ((dist >= 0) & (dist % 4 == 0) & (dist <= 512)).astype(np.float32)
    m += ((dist >= 0) & (dist % 16 == 0) & (dist <= 2048)).astype(np.float32)
    return m.astype(ml_dtypes.bfloat16)


def _pk(v):
    return np.ascontiguousarray(np.asarray(v, np.float32).reshape(8, 128).T)


def prepare_shared(inp):
    f = lambda a: np.ascontiguousarray(np.asarray(a, np.float32))
    sh = {
        "w_in": f(inp["w_in"][0]),
        "w_out": f(inp["w_out"][0]),
        "w_qT": f(np.asarray(inp["w_query"][0]).T),
        "keysT": f(np.stack([np.asarray(inp["sub_keys_a"][0]).T, np.asarray(inp["sub_keys_b"][0]).T])),
        "g_mix": _pk(inp["mix_norm_g"][0]),
        "g_mo": _pk(np.concatenate([np.asarray(inp["attn_out_g"][0]), np.asarray(inp["gate_out_g"][0])])),
        "g_ffn": _pk(inp["ffn_norm_g"][0]),
        "g_q2": f(np.tile(np.asarray(inp["q_norm_g"][0]), 2).reshape(128, 1)),
        "g_k2": f(np.tile(np.asarray(inp["k_norm_g"][0]), 2).reshape(128, 1)),
        "g_vn": f(np.broadcast_to(np.asarray(inp["v_gate_norm_g"][0]).reshape(1, 512), (128, 512))),
        "w_spT": f(np.asarray(inp["w_spatial"][0]).transpose(2, 0, 1)),
        "b_sp": f(np.asarray(inp["b_spatial"][0]).T),
        "tril": f(np.triu(np.ones((128, 128), np.float32))),
        "amask": _attn_mask(),
        "ident_bf": np.eye(128).astype(ml_dtypes.bfloat16),
        "ident_f": np.eye(128, dtype=np.float32),
        "iota_bf": np.broadcast_to(np.arange(128, dtype=np.float32), (128, 128)).astype(ml_dtypes.bfloat16),
        "iota16": f(np.broadcast_to(np.arange(16, dtype=np.float32), (128, 16))),
        "e_uT": f(np.asarray(inp["expert_u"][0]).reshape(NCH, 128, 8, 128).transpose(0, 3, 2, 1).reshape(NCH, 128, D)),
        "e_v": f(inp["expert_v"][0]),
    }
    return sh


_CACHE = {}


def kernel(**inputs):
    x = np.asarray(inputs["x"], np.float32)
    B, S, _ = x.shape
    nseq = B // N_CORES
    key = (nseq, S)
    if key not in _CACHE:
        _CACHE[key] = build_program(nseq, S)
    nc = _CACHE[key]
    sh = prepare_shared(inputs)
    in_maps = []
    for c in range(N_CORES):
        m = dict(sh)
        m["x"] = np.ascontiguousarray(x[c * nseq:(c + 1) * nseq].reshape(nseq * S, D))
        in_maps.append(m)
    res = run_bass_kernel_spmd(nc, in_maps, core_ids=list(range(N_CORES)))
    outs = [np.asarray(r["out"]).reshape(nseq, S, D) for r in res.results]
    return np.concatenate(outs, axis=0).astype(np.float32)
```

```python
import numpy as np
import ml_dtypes
from contextlib import ExitStack

import concourse.bass as bass
import concourse.mybir as mybir
from concourse.bass_utils import run_bass_kernel_spmd

F32 = mybir.dt.float32
BF = mybir.dt.bfloat16
U32 = mybir.dt.uint32
AF = mybir.ActivationFunctionType
ALU = mybir.AluOpType
AX = mybir.AxisListType

D = 1024
NCH = 128
EPS = 1e-6
N_CORES = 8


class Res:
    __slots__ = ("w", "rd")

    def __init__(self):
        self.w = None
        self.rd = []


class Op:
    __slots__ = ("eng", "fn", "deps", "inc", "semv", "is_dma", "dsem", "dval")

    def __init__(self, eng, fn, deps, is_dma=False):
        self.eng = eng
        self.fn = fn
        self.deps = deps
        self.inc = False
        self.semv = None
        self.is_dma = is_dma
        self.dsem = None
        self.dval = None


class Sched:
    ALL = ("pe", "act", "dve", "pool", "sp")

    def __init__(self, nc, n_dma_sems=24):
        self.nc = nc
        self.ops = {e: [] for e in self.ALL}
        self.sems = {e: nc.alloc_semaphore(f"s_{e}") for e in self.ALL}
        self.dma_sems = [nc.alloc_semaphore(f"s_dma{i}") for i in range(n_dma_sems)]
        self.dma_cnt = [0] * n_dma_sems
        self.dma_last = [None] * n_dma_sems
        self.dma_rr = 0
        self.dry = False

    def _deps(self, eng, reads, writes):
        deps = []
        recent = ()
        if eng != "pe":
            recent = self.ops[eng][-3:]
        for r in reads:
            if r.w is not None:
                deps.append(r.w)
        for w in writes:
            if w.w is not None and (w.w.is_dma or w.w.eng != eng or any(w.w is q for q in recent)):
                deps.append(w.w)
            for o in w.rd:
                if o.is_dma or o.eng != eng or any(o is q for q in recent):
                    deps.append(o)
        return deps

    def _commit(self, op, reads, writes):
        for r in reads:
            r.rd.append(op)
        for w in writes:
            w.w = op
            w.rd = []

    def op(self, eng, fn, reads=(), writes=()):
        if self.dry:
            return None
        o = Op(eng, fn, self._deps(eng, reads, writes))
        self.ops[eng].append(o)
        self._commit(o, reads, writes)
        return o

    def dma(self, fn, reads=(), writes=(), eng="sp"):
        if self.dry:
            return None
        deps = self._deps(eng, reads, writes)
        s = self.dma_rr
        self.dma_rr = (self.dma_rr + 1) % len(self.dma_sems)
        if self.dma_last[s] is not None:
            deps.append(self.dma_last[s])
        o = Op(eng, fn, deps, is_dma=True)
        self.dma_cnt[s] += 1
        o.dsem = s
        o.dval = 16 * self.dma_cnt[s]
        self.dma_last[s] = o
        self.ops[eng].append(o)
        self._commit(o, reads, writes)
        return o

    def barrier(self, engines=None):
        lasts = []
        for e in self.ALL:
            for o in reversed(self.ops[e]):
                if not o.is_dma and o.fn is not None:
                    lasts.append(o)
                    break
        lasts += [o for o in self.dma_last if o is not None]
        for e in (engines or self.ALL):
            self.ops[e].append(Op(e, None, list(lasts)))

    def emit(self, block):
        for e in self.ALL:
            for o in self.ops[e]:
                for d in o.deps:
                    if not d.is_dma:
                        d.inc = True
        for e in self.ALL:
            c = 0
            for o in self.ops[e]:
                if not o.is_dma and o.inc:
                    c += 1
                    o.semv = c
        sems, dma_sems = self.sems, self.dma_sems

        def build(e):
            def body(engine):
                seen = {}
                for o in self.ops[e]:
                    need = {}
                    for d in o.deps:
                        if d.is_dma:
                            k, v = ("d", d.dsem), d.dval
                        else:
                            k, v = ("e", d.eng), d.semv
                        if seen.get(k, 0) >= v:
                            continue
                        if need.get(k, 0) < v:
                            need[k] = v
                    for k, v in need.items():
                        engine.wait_ge(dma_sems[k[1]] if k[0] == "d" else sems[k[1]], v)
                        seen[k] = v
                    if o.fn is None:
                        continue
                    ins = o.fn(engine)
                    if o.is_dma:
                        ins.then_inc(dma_sems[o.dsem], 16)
                    elif o.inc:
                        ins.then_inc(sems[e], 1)
            return body

        block.tensor(build("pe"))
        block.scalar(build("act"))
        block.vector(build("dve"))
        block.gpsimd(build("pool"))
        block.sync(build("sp"))


def build_program(nseq, S, dbg=False):
    NT = S // 128
    NTOK = nseq * S
    nc = bass.Bass("TRN2", target_bir_lowering=False)

    def din(name, shape, dt=F32):
        return nc.dram_tensor(name, list(shape), dt, kind="ExternalInput").ap()

    x = din("x", [NTOK, D])
    w_in = din("w_in", [D, 2560])
    w_out = din("w_out", [D, D])
    w_qT = din("w_qT", [2048, D])
    keysT = din("keysT", [2, 128, 128])
    g_mix = din("g_mix", [128, 8])
    g_mo = din("g_mo", [128, 8])
    g_ffn = din("g_ffn", [128, 8])
    g_q2 = din("g_q2", [128, 1])
    g_k2 = din("g_k2", [128, 1])
    g_vn = din("g_vn", [128, 512])
    w_spT = din("w_spT", [128, 4, 128])
    b_sp = din("b_sp", [128, 4])
    tril = din("tril", [128, 128])
    amask_d = din("amask", [128, 17, 128], BF)
    ident_bf_d = din("ident_bf", [128, 128], BF)
    ident_f_d = din("ident_f", [128, 128])
    iota_bf_d = din("iota_bf", [128, 128], BF)
    iota16_d = din("iota16", [128, 16])
    e_uT = din("e_uT", [NCH, 128, D])
    e_v = din("e_v", [NCH * 128, D])
    out = nc.dram_tensor("out", [NTOK, D], F32, kind="ExternalOutput").ap()
    if dbg:
        dbg_x1 = nc.dram_tensor("dbg_x1", [NTOK, D], F32, kind="ExternalOutput").ap()
    x1s = nc.dram_tensor("x1s", [NTOK, D], F32).ap()
    utb = nc.dram_tensor("utb", [NCH, 128, D], BF).ap()
    vbb = nc.dram_tensor("vbb", [NCH, 128, D], BF).ap()

    S_ = Sched(nc)
    op, dma = S_.op, S_.dma
    dbg_outs = {}

    def dump(name, ap, res, T=0, want=0):
        if not dbg or T != want:
            return
        shp = [int(v) for v in ap.shape]
        t = nc.dram_tensor("dbg_" + name, shp, ap.dtype, kind="ExternalOutput").ap()
        dma(lambda e: e.dma_start(out=t, in_=ap), reads=res if isinstance(res, list) else [res])

    def rsqrt_chain(dst, src, n, reads_r, r_dst):
        op("dve", lambda e: e.tensor_scalar(out=dst, in0=src, scalar1=1.0 / n, scalar2=EPS, op0=ALU.mult, op1=ALU.add),
           reads=reads_r, writes=[r_dst])
        w_ = int(dst.shape[1])
        op("pool", lambda e: e.tensor_tensor(out=dst, in0=dst, in1=mhalf[:, 0:w_], op=ALU.pow), reads=[r_dst, r_c], writes=[r_dst])

    def mm(out_, lhsT, rhs, start, stop, reads, writes):
        op("pe", lambda e: e.matmul(out=out_, lhsT=lhsT, rhs=rhs, start=start, stop=stop), reads, writes)

    def tr(out_, in_, ident, reads, writes):
        op("pe", lambda e: e.transpose(out=out_, in_=in_, identity=ident), reads, writes)

    def actf(out_, in_, func, reads, writes, scale=None):
        if scale is None:
            op("act", lambda e: e.activation(out=out_, in_=in_, func=func), reads, writes)
        else:
            op("act", lambda e: e.activation(out=out_, in_=in_, func=func, scale=scale), reads, writes)

    def tt_(eng, out_, in0, in1, alu, reads, writes):
        op(eng, lambda e: e.tensor_tensor(out=out_, in0=in0, in1=in1, op=alu), reads, writes)

    def ts_(eng, out_, in0, s1, s2, op0, op1, reads, writes):
        if op1 is None:
            op(eng, lambda e: e.tensor_scalar(out=out_, in0=in0, scalar1=s1, scalar2=None, op0=op0), reads, writes)
        else:
            op(eng, lambda e: e.tensor_scalar(out=out_, in0=in0, scalar1=s1, scalar2=s2, op0=op0, op1=op1), reads, writes)

    def red(out_, in_, reads, writes):
        op("dve", lambda e: e.tensor_reduce(out=out_, in_=in_, axis=AX.X, op=ALU.add), reads, writes)

    def cp(eng, out_, in_, reads, writes):
        op(eng, lambda e: e.tensor_copy(out=out_, in_=in_), reads, writes)

    def ld(out_, in_, writes, reads=()):
        dma(lambda e: e.dma_start(out=out_, in_=in_), reads=reads, writes=writes)


    with ExitStack() as top:
        def sb(name, shape, dt, stack=top):
            return stack.enter_context(nc.sbuf_tensor("sb_" + name, list(shape), dt))

        def ps(name, shape, dt, stack):
            return stack.enter_context(nc.psum_tensor("ps_" + name, list(shape), dt))

        ident_bf = sb("ident_bf", [128, 128], BF); r_c = Res()
        ident_f = sb("ident_f", [128, 128], F32)
        iota_bf = sb("iota_bf", [128, 128], BF)
        iota16 = sb("iota16", [128, 16], F32)
        mhalf = sb("mhalf", [128, 16], F32)
        gmix = sb("gmix", [128, 8], F32)
        gmo = sb("gmo", [128, 8], F32)
        gffn = sb("gffn", [128, 8], F32)
        gq2 = sb("gq2", [128, 1], F32)
        gqk = sb("gqk", [128, 1], F32)
        bsp = sb("bsp", [128, 4], F32)
        for dst, src in ((ident_bf, ident_bf_d), (ident_f, ident_f_d), (iota_bf, iota_bf_d), (iota16, iota16_d),
                         (gmix, g_mix), (gmo, g_mo), (gffn, g_ffn), (gq2, g_q2), (gqk, g_k2), (bsp, b_sp)):
            dma(lambda e, dst=dst, src=src: e.dma_start(out=dst[:], in_=src), writes=[r_c])
        op("dve", lambda e: e.tensor_tensor(out=gqk[:], in0=gqk[:], in1=gq2[:], op=ALU.mult), reads=[r_c], writes=[r_c])
        op("pool", lambda e: e.memset(mhalf[:], -0.5), writes=[r_c])

        with ExitStack() as pm:
            w_in_b = sb("w_in_b", [128, 8, 2560], BF, pm); r_win = Res()
            w_out_b = sb("w_out_b", [128, 8, D], BF, pm); r_wout = Res()
            wsT = sb("wsT", [128, 4, 128], BF, pm); r_wsT = Res()
            gvn = sb("gvn", [128, 512], F32, pm)
            amask = sb("amask", [128, 17, 128], BF, pm)
            dma(lambda e: e.dma_start(out=gvn[:], in_=g_vn), writes=[r_c])
            dma(lambda e: e.dma_start(out=amask[:], in_=amask_d), writes=[r_c])
            with ExitStack() as pw:
                wst = [sb(f"wst{i}", [128, 2560], F32, pw) for i in range(2)]
                r_wst = [Res(), Res()]
                trl = sb("trl", [128, 128], F32, pw)
                wspf = sb("wspf", [128, 4, 128], F32, pw)
                dma(lambda e: e.dma_start(out=trl[:], in_=tril), writes=[r_c])
                dma(lambda e: e.dma_start(out=wspf[:], in_=w_spT), writes=[r_c])
                op("dve", lambda e: e.tensor_tensor(out=wsT[:], in0=wspf[:], in1=trl[:].unsqueeze(1).to_broadcast([128, 4, 128]),
                                                    op=ALU.mult), reads=[r_c], writes=[r_wsT])
                for k in range(8):
                    s = k % 2
                    dma(lambda e, k=k, s=s: e.dma_start(out=wst[s][:], in_=w_in[k * 128:(k + 1) * 128, :]), writes=[r_wst[s]])
                    op("act", lambda e, k=k, s=s: e.activation(out=w_in_b[:, k, :], in_=wst[s][:], func=AF.Identity,
                                                               scale=gmix[:, k:k + 1]), reads=[r_wst[s], r_c], writes=[r_win])
                for k in range(8):
                    s = k % 2
                    dma(lambda e, k=k, s=s: e.dma_start(out=wst[s][:, 0:D], in_=w_out[k * 128:(k + 1) * 128, :]), writes=[r_wst[s]])
                    op("act", lambda e, k=k, s=s: e.activation(out=w_out_b[:, k, :], in_=wst[s][:, 0:D], func=AF.Identity,
                                                               scale=gmo[:, k:k + 1]), reads=[r_wst[s], r_c], writes=[r_wout])
                S_.barrier()

            xt = [sb(f"xt{i}", [128, D], F32, pm) for i in range(3)]; r_xt = [Res(), Res(), Res()]
            ssx = sb("ssx", [128, 1], F32, pm); r_ssx = Res()
            ss2 = sb("ss2", [128, 2], F32, pm); r_ss2 = Res()
            rstd = sb("rstd", [128, 1], F32, pm); r_rstd = Res()
            xb = sb("xb", [128, D], BF, pm); r_xb = Res()
            xT = sb("xT", [128, 8, 128], BF, pm); r_xT = Res()
            sq = sb("sq", [128, D], F32, pm); r_sq = Res()
            sq2 = sb("sq2", [128, 512], F32, pm); r_sq2 = Res()
            ss16 = sb("ss16", [128, 16], F32, pm); r_ss16 = Res()
            rs16 = sb("rs16", [128, 16], F32, pm); r_rs16 = Res()
            qkn = sb("qkn", [128, D], BF, pm); r_qkn = Res()
            qT = [sb(f"qT{i}", [128, 4, 128], BF, pm) for i in range(2)]; r_qT = [Res(), Res()]
            kT = sb("kT", [128, 4, S], BF, pm); r_kT = [Res() for _ in range(NT)]
            Va = sb("Va", [128, NT, 8, 65], BF, pm); r_V = [Res() for _ in range(NT)]
            ug = [sb(f"ug{i}", [128, 512], F32, pm) for i in range(2)]; r_ug = [Res(), Res()]
            vg = sb("vg", [128, 512], F32, pm); r_vg = Res()
            ss4 = sb("ss4", [128, 4], F32, pm); r_ss4 = Res()
            rs4 = sb("rs4", [128, 4], F32, pm); r_rs4 = Res()
            vgs = sb("vgs", [128, 512], F32, pm); r_vgs = Res()
            vgn = [sb(f"vgn{i}", [128, 512], BF, pm) for i in range(2)]; r_vgn = [Res(), Res()]
            gated = sb("gated", [128, 512], F32, pm); r_gated = Res()
            ssg = sb("ssg", [128, 1], F32, pm); r_ssg = Res()
            rsg = sb("rsg", [128, 1], F32, pm); r_rsg = Res()
            NP = 5
            Pb = sb("Pb", [128, NP, 4, 128], BF, pm); r_P = [Res() for _ in range(NP)]
            rden = sb("rden", [128, 8], F32, pm); r_rden = Res()
            attf = sb("attf", [128, 512], F32, pm); r_attf = Res()
            ssa = sb("ssa", [128, 1], F32, pm); r_ssa = Res()
            rsa = sb("rsa", [128, 1], F32, pm); r_rsa = Res()
            merged = sb("merged", [128, D], BF, pm); r_mg = Res()
            mT = sb("mT", [128, 8, 128], BF, pm); r_mT = Res()
            x1 = [sb(f"x1_{i}", [128, D], F32, pm) for i in range(2)]; r_x1 = [Res(), Res()]

            psT = ps("psT", [128, 8, 128], BF, pm); r_psT = Res()
            NSS = 3
            psS = ps("psS", [128, NSS, 4, 128], F32, pm); r_psS = [Res() for _ in range(NSS)]
            psOt = ps("psO", [128, 8, 128], F32, pm); r_pO = [Res(), Res()]
            psO = psOt[:]
            Fp = ps("Fp", [128, 1024], F32, pm); r_F = [Res(), Res()]
            psZ = Fp[:, 0:512].rearrange("p (g c) -> p g c", c=128)

            ust = [sb(f"ust{i}", [128, D], F32, pm) for i in range(2)]; vst = [sb(f"vst{i}", [128, D], F32, pm) for i in range(2)]
            ubs = [sb("ubs0", [128, D], BF, pm)] * 2; vbs = [sb("vbs0", [128, D], BF, pm)] * 2
            r_ust = [Res(), Res()]; r_vst = [Res(), Res()]; r_ubs = [Res()] * 2; r_vbs = [Res()] * 2

            def tab_load(g):
                sl_ = g % 2
                ld(ust[sl_][:], e_uT[g], [r_ust[sl_]])
                ld(vst[sl_][:], e_v[128 * g:128 * g + 128, :], [r_vst[sl_]])

            def tab_conv(g):
                sl_ = g % 2
                for hh in range(2):
                    tt_("dve", ubs[sl_][:, hh * 512:(hh + 1) * 512].rearrange("p (k e) -> p k e", e=128),
                        ust[sl_][:, hh * 512:(hh + 1) * 512].rearrange("p (k e) -> p k e", e=128),
                        gffn[:, hh * 4:(hh + 1) * 4].unsqueeze(2).to_broadcast([128, 4, 128]), ALU.mult, [r_ust[sl_], r_c], [r_ubs[sl_]])
                cp("pool", vbs[sl_][:], vst[sl_][:], [r_vst[sl_]], [r_vbs[sl_]])
                ld(utb[g], ubs[sl_][:], [], reads=[r_ubs[sl_]])
                ld(vbb[g], vbs[sl_][:], [], reads=[r_vbs[sl_]])

            op("pool", lambda e: e.memset(Va[:, :, :, 64:65], 1.0), writes=r_V)

            tiles = [(s, i) for s in range(nseq) for i in range(NT)]

            def front_gen(T):
                s, i = tiles[T]
                r0 = s * S + i * 128
                xs = xt[T % 3]; rx = r_xt[T % 3]
                sl = T % 2
                ld(xs[:], x[r0:r0 + 128, :], [rx])
                yield
                for hh in range(2):
                    tt_("dve", sq[:, hh * 512:(hh + 1) * 512], xs[:, hh * 512:(hh + 1) * 512], xs[:, hh * 512:(hh + 1) * 512], ALU.mult,
                        [rx], [r_sq])
                    yield
                    cp("dve", xb[:, hh * 512:(hh + 1) * 512], xs[:, hh * 512:(hh + 1) * 512], [rx], [r_xb])
                    yield
                for hh in range(2):
                    red(ss2[:, hh:hh + 1], sq[:, hh * 512:(hh + 1) * 512], [r_sq], [r_ss2])
                    yield
                red(ssx[:], ss2[:], [r_ss2], [r_ssx])
                rsqrt_chain(rstd[:], ssx[:], D, [r_ssx], r_rstd)
                for k in range(8):
                    tr(psT[:, k, :], xb[:, k * 128:(k + 1) * 128], ident_bf[:], [r_xb, r_c], [r_psT])
                yield
                for hh in range(2):
                    cp("dve", xT[:, hh * 4:(hh + 1) * 4, :], psT[:, hh * 4:(hh + 1) * 4, :], [r_psT], [r_xT])
                    yield
                for n in range(2):
                    for k in range(8):
                        mm(Fp[:, n * 512:(n + 1) * 512], xT[:, k, :], w_in_b[:, k, n * 512:(n + 1) * 512], k == 0, k == 7,
                           [r_xT, r_win], [r_F[n]])
                    yield
                actf(sq[:], Fp[:, 0:1024], AF.Square, [r_F[0], r_F[1], r_rstd], [r_sq], scale=rstd[:])
                yield
                for hh in range(2):
                    red(ss16[:, hh * 8:(hh + 1) * 8], sq[:, hh * 512:(hh + 1) * 512].rearrange("p (h c) -> p h c", c=64), [r_sq], [r_ss16])
                    yield
                rsqrt_chain(rs16[:], ss16[:], 64, [r_ss16], r_rs16)
                ts_("dve", rs16[:], rs16[:], rstd[:], None, ALU.mult, None, [r_rs16, r_rstd], [r_rs16])
                yield
                for hh in range(2):
                    tt_("dve", qkn[:, hh * 512:(hh + 1) * 512].rearrange("p (h c) -> p h c", c=64),
                        Fp[:, hh * 512:(hh + 1) * 512].rearrange("p (h c) -> p h c", c=64),
                        rs16[:, hh * 8:(hh + 1) * 8].unsqueeze(2).to_broadcast([128, 8, 64]), ALU.mult, [r_F[hh], r_rs16], [r_qkn])
                    yield
                for n in range(2):
                    for k in range(8):
                        mm(Fp[:, n * 512:(n + 1) * 512], xT[:, k, :], w_in_b[:, k, (2 + n) * 512:(3 + n) * 512], k == 0, k == 7,
                           [r_xT, r_win], [r_F[n]])
                    yield
                ts_("dve", Va[:, i, :, 0:64], Fp[:, 0:512].rearrange("p (h c) -> p h c", c=64), rstd[:], None, ALU.mult, None,
                    [r_F[0], r_rstd], [r_V[i]])
                actf(ug[sl][:], Fp[:, 512:1024], AF.Gelu, [r_F[1], r_rstd], [r_ug[sl]], scale=rstd[:])
                yield
                for k in range(8):
                    mm(Fp[:, 0:512], xT[:, k, :], w_in_b[:, k, 2048:2560], k == 0, k == 7, [r_xT, r_win], [r_F[0]])
                yield
                for k in range(8):
                    tr(psT[:, k, :], qkn[:, k * 128:(k + 1) * 128], ident_bf[:], [r_qkn, r_c], [r_psT])
                yield
                cp("dve", qT[sl][:], psT[:, 0:4, :], [r_psT], [r_qT[sl]])
                yield
                ts_("dve", kT[:, :, i * 128:(i + 1) * 128], psT[:, 4:8, :], gqk[:], None, ALU.mult, None, [r_psT, r_c], [r_kT[i]])
                yield
                actf(vg[:], Fp[:, 0:512], AF.Gelu, [r_F[0], r_rstd], [r_vg], scale=rstd[:])
                yield
                tt_("dve", sq[:, 0:512], vg[:], vg[:], ALU.mult, [r_vg], [r_sq])
                yield
                red(ss4[:], sq[:, 0:512].rearrange("p (g c) -> p g c", c=128), [r_sq], [r_ss4])
                rsqrt_chain(rs4[:], ss4[:], 128, [r_ss4], r_rs4)
                yield
                tt_("dve", vgs[:].rearrange("p (g c) -> p g c", c=128), vg[:].rearrange("p (g c) -> p g c", c=128),
                    rs4[:].unsqueeze(2).to_broadcast([128, 4, 128]), ALU.mult, [r_vg, r_rs4], [r_vgs])
                yield
                tt_("pool", vgn[sl][:], vgs[:], gvn[:], ALU.mult, [r_vgs, r_c], [r_vgn[sl]])
                yield

            cnt = {"ps": 0, "p": 0}

            def attn(T, filler, n_f):
                s, i = tiles[T]
                r0 = s * S + i * 128
                sl = T % 2
                lo = max(0, i - 16)
                kts = list(range(lo, i + 1))
                items = [(h, kts[a:a + 4]) for h in range(8) for a in range(0, len(kts), 4)]

                def emit_st(it):
                    h, batch = it
                    ssl = cnt["ps"] % NSS; cnt["ps"] += 1
                    pl = cnt["p"] % NP; cnt["p"] += 1
                    pr, hp = h // 2, h % 2
                    for b_, j in enumerate(batch):
                        mm(psS[:, ssl, b_, :], kT[hp * 64:(hp + 1) * 64, pr, j * 128:(j + 1) * 128], qT[sl][hp * 64:(hp + 1) * 64, pr, :],
                           True, True, [r_kT[j], r_qT[sl]], [r_psS[ssl]])
                    nb = len(batch)
                    op("act", lambda e, ssl=ssl, pl=pl, nb=nb: e.activation(out=Pb[:, pl, 0:nb, :], in_=psS[:, ssl, 0:nb, :],
                                                                            func=AF.Exp, scale=0.125), [r_psS[ssl]], [r_P[pl]])
                    idx0 = 16 - (i - batch[0])
                    tt_("dve", Pb[:, pl, 0:nb, :], Pb[:, pl, 0:nb, :], amask[:, idx0:idx0 + nb, :], ALU.mult, [r_P[pl], r_c], [r_P[pl]])
                    return pl

                def emit_pv(it, pl):
                    h, batch = it
                    for b_, j in enumerate(batch):
                        mm(psO[:, h, 0:65], Pb[:, pl, b_, :], Va[:, j, h, :], j == kts[0], j == kts[-1],
                           [r_P[pl], r_V[j]], [r_pO[h // 4]])

                gpt = -(-NCH // len(tiles))
                my_groups = [g for g in range(T * gpt, min(NCH, (T + 1) * gpt))]
                for g in my_groups[:2]:
                    tab_load(g)
                pace = n_f / max(1, len(items))
                acc = 0.0
                pls = [emit_st(it_) for it_ in items[:2]]
                for n in range(len(items)):
                    if n + 2 < len(items):
                        pls.append(emit_st(items[n + 2]))
                    emit_pv(items[n], pls[n])
                    acc += pace
                    while filler is not None and acc >= 1.0:
                        acc -= 1.0
                        if next(filler, "done") == "done":
                            filler = None
                if filler is not None:
                    for _ in filler:
                        pass
                op("dve", lambda e: e.reciprocal(out=rden[:], in_=psO[:, :, 64]), [r_pO[0], r_pO[1]], [r_rden])
                tt_("dve", attf[:].rearrange("p (h c) -> p h c", c=64), psO[:, :, 0:64],
                    rden[:].unsqueeze(2).to_broadcast([128, 8, 64]), ALU.mult, [r_pO[0], r_pO[1], r_rden], [r_attf])
                for n_, g in enumerate(my_groups):
                    tab_conv(g)
                    if n_ + 2 < len(my_groups):
                        tab_load(my_groups[n_ + 2])

            def tail_gen(T):
                s, i = tiles[T]
                r0 = s * S + i * 128
                xs = xt[T % 3]; rx = r_xt[T % 3]
                sl = T % 2
                tt_("dve", sq2[:], attf[:], attf[:], ALU.mult, [r_attf], [r_sq2])
                yield
                red(ssa[:], sq2[:], [r_sq2], [r_ssa])
                rsqrt_chain(rsa[:], ssa[:], 512, [r_ssa], r_rsa)
                yield
                ts_("dve", merged[:, 0:512], attf[:], rsa[:], None, ALU.mult, None, [r_attf, r_rsa], [r_mg])
                yield
                for g in range(4):
                    mm(psZ[:, g, :], wsT[:, g, :], vgn[sl][:, g * 128:(g + 1) * 128], True, True, [r_wsT, r_vgn[sl]], [r_F[0]])
                for g in range(4):
                    op("dve", lambda e, g=g, sl=sl: e.scalar_tensor_tensor(out=gated[:, g * 128:(g + 1) * 128], in0=psZ[:, g, :],
                                                                           scalar=bsp[:, g:g + 1], in1=ug[sl][:, g * 128:(g + 1) * 128],
                                                                           op0=ALU.add, op1=ALU.mult),
                       [r_F[0], r_ug[sl], r_c], [r_gated])
                yield
                tt_("dve", sq2[:], gated[:], gated[:], ALU.mult, [r_gated], [r_sq2])
                yield
                red(ssg[:], sq2[:], [r_sq2], [r_ssg])
                rsqrt_chain(rsg[:], ssg[:], 512, [r_ssg], r_rsg)
                yield
                ts_("dve", merged[:, 512:1024], gated[:], rsg[:], None, ALU.mult, None, [r_gated, r_rsg], [r_mg])
                yield
                for k in range(8):
                    tr(psT[:, k, :], merged[:, k * 128:(k + 1) * 128], ident_bf[:], [r_mg, r_c], [r_psT])
                yield
                for hh in range(2):
                    cp("dve", mT[:, hh * 4:(hh + 1) * 4, :], psT[:, hh * 4:(hh + 1) * 4, :], [r_psT], [r_mT])
                    yield
                xo = x1[T % 2]; rxo = r_x1[T % 2]
                for n in range(2):
                    bk = 1 - n
                    for k in range(8):
                        mm(Fp[:, bk * 512:(bk + 1) * 512], mT[:, k, :], w_out_b[:, k, n * 512:(n + 1) * 512], k == 0, k == 7,
                           [r_mT, r_wout], [r_F[bk]])
                    yield
                    tt_("dve", xo[:, n * 512:(n + 1) * 512], Fp[:, bk * 512:(bk + 1) * 512], xs[:, n * 512:(n + 1) * 512], ALU.add,
                        [r_F[bk], rx], [rxo])
                ld(x1s[r0:r0 + 128, :], xo[:], [], reads=[rxo])
                if dbg:
                    ld(dbg_x1[r0:r0 + 128, :], xo[:], [], reads=[rxo])
                yield

            for _ in front_gen(0):
                pass

            def chain(*gens):
                for g_ in gens:
                    if g_ is not None:
                        for v_ in g_:
                            yield v_

            nt_ = len(tiles)
            for T in range(nt_):
                tg = tail_gen(T - 1) if T > 0 else None
                fg = front_gen(T + 1) if T + 1 < nt_ else None
                n_f = (14.0 if T > 0 else 0.0) + (34.0 if fg is not None else 0.0)
                if fg is not None:
                    next(fg)
                attn(T, chain(tg, fg), n_f)
            for _ in tail_gen(nt_ - 1):
                pass
            S_.barrier()

        with ExitStack() as pp:
            WS = sb("WS", [128, 8, 2048], BF, pp); r_WS = Res()
            with ExitStack() as pw:
                kTt = sb("kTt", [128, 2, 128], F32, pw)
                wq = [sb(f"wq{i}", [128, D], F32, pw) for i in range(2)]; r_wq = [Res(), Res()]
                psW0 = ps("psW0", [128, 8, 128], F32, pw); r_psW0 = Res()
                dma(lambda e: e.dma_start(out=kTt[:], in_=keysT.rearrange("s c k -> c s k")), writes=[r_c])
                for hs in range(16):
                    s = hs % 2
                    dma(lambda e, hs=hs, s=s: e.dma_start(out=wq[s][:], in_=w_qT[hs * 128:(hs + 1) * 128, :]), writes=[r_wq[s]])
                    for k in range(8):
                        op("pe", lambda e, hs=hs, s=s, k=k: e.matmul(out=psW0[:, k, :], lhsT=wq[s][:, k * 128:(k + 1) * 128],
                                                                     rhs=kTt[:, hs % 2, :], start=True, stop=True),
                           reads=[r_wq[s], r_c], writes=[r_psW0])
                    op("dve", lambda e, hs=hs: e.tensor_tensor(out=WS[:, :, hs * 128:(hs + 1) * 128], in0=psW0[:],
                                                               in1=gffn[:].unsqueeze(2).to_broadcast([128, 8, 128]), op=ALU.mult),
                       reads=[r_psW0, r_c], writes=[r_WS])
                S_.barrier()

            TB = 256
            NB = NTOK // TB
            GC = 2
            NG = NCH // GC

            x1p = [sb("x1p0", [128, D], F32, pp)] * 2; r_x1p = [Res()] * 2
            x1r = sb("x1r", [128, D], F32, pp); r_x1r = Res()
            ssxp = sb("ssxp", [128, 1], F32, pp); r_ssxp = Res()
            rstdp = sb("rstdp", [128, 1], F32, pp); r_rstdp = Res()
            xnb = sb("xnb", [128, D], BF, pp); r_xnb = Res()
            XnT = [sb(f"XnT{i}", [128, 8, TB], BF, pp) for i in range(2)]; r_XnT = [Res(), Res()]
            S1 = sb("S1", [128, 2048], F32, pp); r_S1 = Res()
            S2 = sb("S2", [128, 2048], F32, pp); r_S2 = Res()
            S3 = S1; r_S3 = r_S1
            vab = sb("vab", [128, 8, 2, 16], F32, pp); r_vab = [Res() for _ in range(16)]
            iabu = sb("iabu", [128, 8, 2, 16], U32, pp); r_iabu = [Res() for _ in range(16)]
            r_S2v = [Res() for _ in range(16)]; r_S2c = [Res() for _ in range(8)]
            r_topsh = [Res() for _ in range(8)]; r_posuh = [Res() for _ in range(8)]
            iabf = sb("iabf", [128, 8, 2, 16], F32, pp); r_iabf = Res()
            tops = sb("tops", [128, 8, 16], F32, pp); r_tops = Res()
            posu = sb("posu", [128, 8, 16], U32, pp); r_posu = Res()
            pij_u = sb("pij_u", [128, 2, 128], U32, pp); r_piju = Res()
            pij_f = sb("pij_f", [128, 2, 128], F32, pp); r_pijf = Res()
            sel = sb("sel", [128, 3, 128], F32, pp); r_sel = Res()
            sm8 = sb("sm8", [128, 8], F32, pp); r_sm8 = Res()
            pickT = sb("pickT", [128, 3, 128], BF, pp); r_pickT = Res()
            pickF = sb("pickF", [128, 3, 128], F32, pp)
            TG = 8
            iota3 = sb("iota3", [128, 128, TG], BF, pp)
            Ag = [sb(f"Ag{i}", [128, TG, 128], BF, pp) for i in range(2)]
            Bg = [sb(f"Bg{i}", [128, 128, TG], BF, pp) for i in range(2)]
            r_AB = [Res(), Res()]
            op("dve", lambda e: e.tensor_copy(out=iota3[:], in_=iota_bf[:].unsqueeze(2).to_broadcast([128, 128, TG])),
               reads=[r_c], writes=[r_c])
            WTb = [sb(f"WTb{i}", [128, 128, 128], BF, pp) for i in range(3)]; r_WTb = [Res() for _ in range(3)]
            Gt = sb("Gt", [128, 3, TB], BF, pp); r_G = [Res(), Res(), Res()]
            Ct = sb("Ct", [128, 3, TB], BF, pp); r_C = [[Res(), Res()] for _ in range(3)]
            Ug = [sb(f"Ug{i}", [128, GC, D], BF, pp) for i in range(2)]; r_Ug = [Res(), Res()]
            Vg = [sb(f"Vg{i}", [128, GC, D], BF, pp) for i in range(3)]; r_Vg = [Res(), Res(), Res()]

            psF = ps("psF", [128, 512], F32, pp); r_psF = Res()
            psTq = psF[:].bitcast(BF).rearrange("p (k t) -> p k t", t=128); r_psTq = r_psF
            psWa = ps("psWa", [128, 512], F32, pp); r_psWa = Res()
            psWs = [psWa[:].rearrange("p (t a) -> p t a", a=128), psF[:].rearrange("p (t a) -> p t a", a=128)]
            r_psW = [r_psWa, r_psF]
            big = ps("big", [128, 4, 512], F32, pp); r_big = [Res() for _ in range(4)]
            psH = ps("psH", [128, 2, 512], F32, pp); r_psH = [Res(), Res()]

            S2v = S2[:].rearrange("p (a b) -> p a b", b=128)
            S2c = S2[:].rearrange("p (a b) -> p a b", b=256)
            S2e = S2[:].rearrange("p (a b) -> p a b", b=16)
            S3c = S3[:].rearrange("p (a b) -> p a b", b=256)

            def prep_gen(b):
                xsl = b % 2
                for tt in range(2):
                    gt = 2 * b + tt
                    wsl = gt % 3
                    xs = x1p[gt % 2]; rx = r_x1p[gt % 2]
                    r0 = b * TB + tt * 128
                    ld(xs[:], x1s[r0:r0 + 128, :], [rx])
                    yield 0.5
                    yield 0.5
                    actf(S3[:, 0:D], xs[:], AF.Square, [rx], [r_S3])
                    yield 1.0
                    red(ssxp[:], S3[:, 0:D], [r_S3], [r_ssxp])
                    rsqrt_chain(rstdp[:], ssxp[:], D, [r_ssxp], r_rstdp)
                    yield 1.2
                    yield 0.8
                    actf(xnb[:], xs[:], AF.Identity, [rx, r_rstdp], [r_xnb], scale=rstdp[:])
                    yield 1.0
                    for k in range(8):
                        tr(psTq[:, k, :], xnb[:, k * 128:(k + 1) * 128], ident_bf[:], [r_xnb, r_c], [r_psTq])
                    yield 0.8
                    actf(XnT[xsl][:, :, tt * 128:(tt + 1) * 128], psTq, AF.Copy, [r_psTq], [r_XnT[xsl]])
                    yield 0.9
                    for n in range(4):
                        for k in range(8):
                            mm(psF[:], XnT[xsl][:, k, tt * 128:(tt + 1) * 128], WS[:, k, n * 512:(n + 1) * 512],
                               k == 0, k == 7, [r_XnT[xsl], r_WS], [r_psF])
                        yield 1.9
                        actf(S1[:, n * 512:(n + 1) * 512], psF[:], AF.Copy, [r_psF], [r_S1])
                        yield 0.8
                    for step in range(5):
                        for hs in range(16):
                            src = S1[:, hs * 128:(hs + 1) * 128]
                            m = vab[:, hs // 2, hs % 2, :]; ix = iabu[:, hs // 2, hs % 2, :]
                            v2 = S2v[:, hs, :]
                            rv_, ri_, r2_ = r_vab[hs], r_iabu[hs], r_S2v[hs]
                            if step == 0:
                                op("dve", lambda e, m=m, src=src: e.max(out=m[:, 0:8], in_=src), [r_S1], [rv_])
                            elif step == 1:
                                op("dve", lambda e, m=m, ix=ix, src=src: e.max_index(out=ix[:, 0:8], in_max=m[:, 0:8], in_values=src),
                                   [r_S1, rv_], [ri_])
                            elif step == 2:
                                op("dve", lambda e, m=m, src=src, v2=v2: e.match_replace(out=v2, in_to_replace=m[:, 0:8], in_values=src,
                                                                                         imm_value=-1e30), [r_S1, rv_], [r2_, r_S2c[hs // 2]])
                            elif step == 3:
                                op("dve", lambda e, m=m, v2=v2: e.max(out=m[:, 8:16], in_=v2), [r2_], [rv_])
                            else:
                                op("dve", lambda e, m=m, ix=ix, v2=v2: e.max_index(out=ix[:, 8:16], in_max=m[:, 8:16], in_values=v2),
                                   [r2_, rv_], [ri_])
                            if hs % 4 == 3:
                                yield 1.0
                    for hp in range(4):
                        tt_("dve", S3[:, hp * 512:(hp + 1) * 512].rearrange("p (h a b) -> p h a b", a=16, b=16),
                            vab[:, 2 * hp:2 * hp + 2, 0, :].unsqueeze(3).to_broadcast([128, 2, 16, 16]),
                            vab[:, 2 * hp:2 * hp + 2, 1, :].unsqueeze(2).to_broadcast([128, 2, 16, 16]), ALU.add, r_vab, [r_S3])
                        yield 0.6
                    for step in range(5):
                        for h in range(8):
                            cf = S3c[:, h, :]; v2 = S2c[:, h, :]
                            tp = tops[:, h, :]; px = posu[:, h, :]
                            rt_, rp_, r2_ = r_topsh[h], r_posuh[h], r_S2c[h]
                            if step == 0:
                                op("dve", lambda e, tp=tp, cf=cf: e.max(out=tp[:, 0:8], in_=cf), [r_S3], [rt_])
                            elif step == 1:
                                op("dve", lambda e, tp=tp, px=px, cf=cf: e.max_index(out=px[:, 0:8], in_max=tp[:, 0:8], in_values=cf),
                                   [r_S3, rt_], [rp_])
                            elif step == 2:
                                op("dve", lambda e, tp=tp, cf=cf, v2=v2: e.match_replace(out=v2, in_to_replace=tp[:, 0:8], in_values=cf,
                                                                                         imm_value=-1e30),
                                   [r_S3, rt_], [r2_, r_S2v[2 * h], r_S2v[2 * h + 1]])
                            elif step == 3:
                                op("dve", lambda e, tp=tp, v2=v2: e.max(out=tp[:, 8:16], in_=v2), [r2_], [rt_])
                            else:
                                op("dve", lambda e, tp=tp, px=px, v2=v2: e.max_index(out=px[:, 8:16], in_max=tp[:, 8:16], in_values=v2),
                                   [r2_, rt_], [rp_])
                            if h % 4 == 3:
                                yield 1.4
                    pf = posu[:].rearrange("p h k -> p (h k)")
                    op("dve", lambda e, pf=pf: e.tensor_single_scalar(out=pij_u[:, 0, :], in_=pf, scalar=4, op=ALU.logical_shift_right),
                       r_posuh, [r_piju])
                    op("dve", lambda e, pf=pf: e.tensor_single_scalar(out=pij_u[:, 1, :], in_=pf, scalar=15, op=ALU.bitwise_and),
                       r_posuh, [r_piju])
                    cp("dve", pij_f[:], pij_u[:], [r_piju], [r_pijf])
                    cp("dve", iabf[:], iabu[:], r_iabu, [r_iabf])
                    yield 0.9
                    for sd in range(2):
                        for hp in range(4):
                            e3 = S2[:, hp * 512:(hp + 1) * 512].rearrange("p (k i) -> p k i", i=16)
                            e4 = S2[:, hp * 512:(hp + 1) * 512].rearrange("p (h k i) -> p h k i", k=16, i=16)
                            tt_("dve", e3, iota16[:].unsqueeze(1).to_broadcast([128, 32, 16]),
                                pij_f[:, sd, hp * 32:(hp + 1) * 32].unsqueeze(2).to_broadcast([128, 32, 16]), ALU.is_equal,
                                [r_pijf, r_c], [r_S2] + r_S2v + r_S2c)
                            tt_("dve", e4, e4, iabf[:, 2 * hp:2 * hp + 2, sd, :].unsqueeze(2).to_broadcast([128, 2, 16, 16]), ALU.mult,
                                [r_S2, r_iabf], [r_S2] + r_S2v + r_S2c)
                            yield 1.2
                            red(sel[:, sd, hp * 32:(hp + 1) * 32], e3, [r_S2] + r_S2v + r_S2c, [r_sel])
                            yield 0.6
                    g3 = sel[:, 2, :].rearrange("p (h k) -> p h k", k=16)
                    tt_("dve", g3, tops[:], tops[:, :, 0:1].to_broadcast([128, 8, 16]), ALU.subtract, r_topsh, [r_sel])
                    yield 0.5
                    actf(sel[:, 2, :], sel[:, 2, :], AF.Exp, [r_sel], [r_sel])
                    yield 0.5
                    red(sm8[:], g3, [r_sel], [r_sm8])
                    op("dve", lambda e: e.reciprocal(out=sm8[:], in_=sm8[:]), [r_sm8], [r_sm8])
                    tt_("dve", g3, g3, sm8[:].unsqueeze(2).to_broadcast([128, 8, 16]), ALU.mult, [r_sel, r_sm8], [r_sel])
                    yield 0.8
                    for q in range(3):
                        tr(psF[:, q * 128:(q + 1) * 128], sel[:, q, :], ident_f[:], [r_sel, r_c], [r_psF])
                    yield 0.5
                    actf(pickT[:].rearrange("p q t -> p (q t)"), psF[:, 0:384], AF.Copy, [r_psF], [r_pickT])
                    actf(pickF[:].rearrange("p q t -> p (q t)"), psF[:, 0:384], AF.Copy, [r_psF], [r_pickT])
                    yield 1.0
                    NGR = 128 // TG
                    if tt == 1:
                        yield "barrier"

                    fastA = (tt == 1)

                    def onehot_b(g):
                        sl = g % 2
                        t0 = g * TG
                        tt_("dve", Bg[sl][:], iota3[:], pickT[:, 1, t0:t0 + TG].unsqueeze(1).to_broadcast([128, 128, TG]),
                            ALU.is_equal, [r_pickT, r_c], [r_AB[sl]])
                        if fastA:
                            a3 = Ag[sl][:].rearrange("p t a -> p (t a)").rearrange("p (a t) -> p a t", t=TG)
                            tt_("dve", a3, iota3[:], pickT[:, 0, t0:t0 + TG].unsqueeze(1).to_broadcast([128, 128, TG]),
                                ALU.is_equal, [r_pickT, r_c], [r_AB[sl]])
                            tt_("dve", a3, a3, pickT[:, 2, t0:t0 + TG].unsqueeze(1).to_broadcast([128, 128, TG]),
                                ALU.mult, [r_pickT, r_AB[sl]], [r_AB[sl]])

                    def onehot_a(g, half):
                        if fastA:
                            return
                        sl = g % 2
                        t0 = g * TG
                        for u in range(half * 4, half * 4 + 4):
                            ts_("dve", Ag[sl][:, u, :], iota_bf[:], pickF[:, 0, t0 + u:t0 + u + 1], pickF[:, 2, t0 + u:t0 + u + 1],
                                ALU.is_equal, ALU.mult, [r_pickT, r_c], [r_AB[sl]])

                    def wmm(q):
                        g = (4 * q) // TG
                        sl = g % 2
                        for u in range(4):
                            ti = (4 * q + u) % TG
                            if fastA:
                                rhs = Ag[sl][:].rearrange("p t a -> p (t a)").rearrange("p (a t) -> p a t", t=TG)[:, :, ti]
                            else:
                                rhs = Ag[sl][:, ti, :]
                            mm(psWs[q % 2][:, u, :], Bg[sl][:, :, ti], rhs, True, True, [r_AB[sl]], [r_psW[q % 2]])

                    def wev(q):
                        actf(WTb[wsl][:, 4 * q:4 * q + 4, :], psWs[q % 2], AF.Copy, [r_psW[q % 2]], [r_WTb[wsl]])

                    NQ = 32
                    for g in range(NGR + 2):
                        if g < NGR:
                            onehot_b(g)
                            onehot_a(g, 0)
                        if 0 <= 2 * g - 2 < NQ:
                            wmm(2 * g - 2)
                        if 0 <= 2 * g - 3 < NQ:
                            wev(2 * g - 3)
                        yield 1.7
                        if g < NGR:
                            onehot_a(g, 1)
                        if 0 <= 2 * g - 1 < NQ:
                            wmm(2 * g - 1)
                        if 0 <= 2 * g - 2 < NQ:
                            wev(2 * g - 2)
                        yield 1.1

            def load_tables(gidx):
                g = gidx % NG
                ld(Ug[gidx % 2][:], utb[g * GC:(g + 1) * GC].rearrange("c p f -> p c f"), [r_Ug[gidx % 2]])
                ld(Vg[gidx % 3][:], vbb[g * GC:(g + 1) * GC].rearrange("c p f -> p c f"), [r_Vg[gidx % 3]])

            def emit_h(b, c, gidx):
                hs = c % 2
                gs = c % 3
                cc = c % GC
                xsl = b % 2
                us = gidx % 2
                for k in range(8):
                    mm(psH[:, hs, 0:TB], Ug[us][:, cc, k * 128:(k + 1) * 128], XnT[xsl][:, k, :], k == 0, k == 7,
                       [r_Ug[us], r_XnT[xsl]], [r_psH[hs]])
                actf(Gt[:, gs, :], psH[:, hs, 0:TB], AF.Gelu, [r_psH[hs]], [r_G[gs]])
                for t2 in range(2):
                    wsl = (2 * b + t2) % 3
                    tt_(C_ENG[t2], Ct[:, gs, t2 * 128:(t2 + 1) * 128], Gt[:, gs, t2 * 128:(t2 + 1) * 128], WTb[wsl][:, :, c],
                        ALU.mult, [r_G[gs], r_WTb[wsl]], [r_C[gs][t2]])

            def emit_o(c, gidx):
                gs = c % 3
                cc = c % GC
                vs = gidx % 3
                for t2 in range(2):
                    for nh in range(2):
                        mm(big[:, t2 * 2 + nh, :], Ct[:, gs, t2 * 128:(t2 + 1) * 128], Vg[vs][:, cc, nh * 512:(nh + 1) * 512],
                           c == 0, c == NCH - 1, [r_C[gs][t2], r_Vg[vs]], [r_big[t2 * 2 + nh]])

            C_ENG = ("pool", "pool")

            S_.dry = True
            n_units = 0.0
            for r_ in prep_gen(1):
                if r_ == "barrier":
                    break
                n_units += r_
            S_.dry = False
            PACE = n_units / float(NCH - 6)

            for _ in prep_gen(0):
                pass
            gcount = 0
            load_tables(0)
            for b in range(NB):
                filler = prep_gen(b + 1) if b + 1 < NB else None
                stalled = False
                acc = 0.0
                slots = {}
                for c in range(NCH):
                    if c % GC == 0:
                        slots[c // GC] = gcount
                        gcount += 1
                    emit_h(b, c, slots[c // GC])
                    if c >= 2:
                        emit_o(c - 2, slots[(c - 2) // GC])
                    if c % GC == 0 and not (b == NB - 1 and c // GC == NG - 1):
                        load_tables(gcount)
                    acc += PACE
                    while filler is not None and not stalled and acc > 0.0:
                        r_ = next(filler, "done")
                        if r_ == "done":
                            filler = None
                        elif r_ == "barrier":
                            stalled = True
                        else:
                            acc -= r_
                emit_o(NCH - 2, slots[(NCH - 2) // GC])
                emit_o(NCH - 1, slots[(NCH - 1) // GC])
                if filler is not None:
                    for _ in filler:
                        pass
                for t2 in range(2):
                    r0 = b * TB + t2 * 128
                    ld(x1r[:], x1s[r0:r0 + 128, :], [r_x1r])
                    tt_("dve", x1r[:], big[:, 2 * t2:2 * t2 + 2, :].rearrange("p n f -> p (n f)"), x1r[:], ALU.add,
                        [r_big[2 * t2], r_big[2 * t2 + 1], r_x1r], [r_x1r])
                    dma(lambda e, r0=r0: e.dma_start(out=out[r0:r0 + 128, :], in_=x1r[:]), reads=[r_x1r])
            S_.barrier()

        with nc.Block() as block:
            S_.emit(block)
    return nc


def _attn_mask():
    k = np.arange(128)[:, None, None]
    idx = np.arange(17)[None, :, None]
    q = np.arange(128)[None, None, :]
    dist = (16 - idx) * 128 + q - k
    m = ((dist >= 0) & (dist <= 128)).astype(np.float32)
    m += ((dist >= 0) & (dist % 4 == 0) & (dist <= 512)).astype(np.float32)
    m += ((dist >= 0) & (dist % 16 == 0) & (dist <= 2048)).astype(np.float32)
    return m.astype(ml_dtypes.bfloat16)


def _pk(v):
    return np.ascontiguousarray(np.asarray(v, np.float32).reshape(8, 128).T)


def prepare_shared(inp):
    f = lambda a: np.ascontiguousarray(np.asarray(a, np.float32))
    sh = {
        "w_in": f(inp["w_in"][0]),
        "w_out": f(inp["w_out"][0]),
        "w_qT": f(np.asarray(inp["w_query"][0]).T),
        "keysT": f(np.stack([np.asarray(inp["sub_keys_a"][0]).T, np.asarray(inp["sub_keys_b"][0]).T])),
        "g_mix": _pk(inp["mix_norm_g"][0]),
        "g_mo": _pk(np.concatenate([np.asarray(inp["attn_out_g"][0]), np.asarray(inp["gate_out_g"][0])])),
        "g_ffn": _pk(inp["ffn_norm_g"][0]),
        "g_q2": f(np.tile(np.asarray(inp["q_norm_g"][0]), 2).reshape(128, 1)),
        "g_k2": f(np.tile(np.asarray(inp["k_norm_g"][0]), 2).reshape(128, 1)),
        "g_vn": f(np.broadcast_to(np.asarray(inp["v_gate_norm_g"][0]).reshape(1, 512), (128, 512))),
        "w_spT": f(np.asarray(inp["w_spatial"][0]).transpose(2, 0, 1)),
        "b_sp": f(np.asarray(inp["b_spatial"][0]).T),
        "tril": f(np.triu(np.ones((128, 128), np.float32))),
        "amask": _attn_mask(),
        "ident_bf": np.eye(128).astype(ml_dtypes.bfloat16),
        "ident_f": np.eye(128, dtype=np.float32),
        "iota_bf": np.broadcast_to(np.arange(128, dtype=np.float32), (128, 128)).astype(ml_dtypes.bfloat16),
        "iota16": f(np.broadcast_to(np.arange(16, dtype=np.float32), (128, 16))),
        "e_uT": f(np.asarray(inp["expert_u"][0]).reshape(NCH, 128, 8, 128).transpose(0, 3, 2, 1).reshape(NCH, 128, D)),
        "e_v": f(inp["expert_v"][0]),
    }
    return sh


_CACHE = {}


def kernel(**inputs):
    x = np.asarray(inputs["x"], np.float32)
    B, S, _ = x.shape
    nseq = B // N_CORES
    key = (nseq, S)
    if key not in _CACHE:
        _CACHE[key] = build_program(nseq, S)
    nc = _CACHE[key]
    sh = prepare_shared(inputs)
    in_maps = []
    for c in range(N_CORES):
        m = dict(sh)
        m["x"] = np.ascontiguousarray(x[c * nseq:(c + 1) * nseq].reshape(nseq * S, D))
        in_maps.append(m)
    res = run_bass_kernel_spmd(nc, in_maps, core_ids=list(range(N_CORES)))
    outs = [np.asarray(r["out"]).reshape(nseq, S, D) for r in res.results]
    return np.concatenate(outs, axis=0).astype(np.float32)
```
